# Optimizing a Trainium2 kernel written in Bass

```python
import math
import jax, jax.numpy as jnp
from jax import lax
import numpy as np

D_MODEL = 1024
BATCH = 4
SEQ = 8192
DEPTH = 4

MEM_LEN = 256
S5_WIDTH = D_MODEL // 2
S5_GROUP_CH = 16
S5_GROUPS = S5_WIDTH // S5_GROUP_CH
S5_STATE = 64
RET_HEADS = 4
RET_DK = (D_MODEL - S5_WIDTH) // RET_HEADS
RET_DV = RET_DK
RET_WIDTH = RET_HEADS * RET_DV
RET_CHUNK = 128
MIX_WIDTH = S5_WIDTH + RET_WIDTH
IN_WIDTH = S5_WIDTH + 4 * RET_WIDTH
X_HEADS = 4
X_HEAD_DIM = D_MODEL // X_HEADS
D_FF = ((8 * D_MODEL // 3 + 255) // 256) * 256
ROPE_BASE = 10000.0
EPS = 1e-6

kernel_name = "hybrid_s5_retention_memxattn_swiglu"


def rms_norm(x, g):
    xf = x.astype(jnp.float32)
    y = xf * lax.rsqrt(jnp.mean(xf * xf, axis=-1, keepdims=True) + EPS)
    return (y * g.astype(jnp.float32)).astype(x.dtype)


def _complex_combine(left, right):
    ar1, ai1, br1, bi1 = left
    ar2, ai2, br2, bi2 = right
    ar = ar2 * ar1 - ai2 * ai1
    ai = ar2 * ai1 + ai2 * ar1
    br = ar2 * br1 - ai2 * bi1 + br2
    bi = ar2 * bi1 + ai2 * br1 + bi2
    return (ar, ai, br, bi)


def s5_mixer(u, lam_re, lam_im, log_step, b_re, b_im, c_re, c_im, d_skip, w_glu, b_glu):
    bsz, seq, _ = u.shape
    uf = u.astype(jnp.float32)
    ug = uf.reshape(bsz, seq, S5_GROUPS, S5_GROUP_CH)
    lr = lam_re.astype(jnp.float32)
    li = lam_im.astype(jnp.float32)
    step = jnp.exp(log_step.astype(jnp.float32))[:, None]
    mag = jnp.exp(lr * step)
    abar_r = mag * jnp.cos(li * step)
    abar_i = mag * jnp.sin(li * step)
    den = lr * lr + li * li
    f_r = ((abar_r - 1.0) * lr + abar_i * li) / den
    f_i = (abar_i * lr - (abar_r - 1.0) * li) / den
    br = b_re.astype(jnp.float32)
    bi = b_im.astype(jnp.float32)
    bb_r = f_r[..., None] * br - f_i[..., None] * bi
    bb_i = f_r[..., None] * bi + f_i[..., None] * br
    xr = jnp.einsum('blgh,gph->blgp', ug, bb_r)
    xi = jnp.einsum('blgh,gph->blgp', ug, bb_i)
    a_r = jnp.broadcast_to(abar_r[None, None], (1, seq) + abar_r.shape)
    a_i = jnp.broadcast_to(abar_i[None, None], (1, seq) + abar_i.shape)
    _, _, sr, si = lax.associative_scan(_complex_combine, (a_r, a_i, xr, xi), axis=1)
    y = (jnp.einsum('gnp,blgp->blgn', c_re.astype(jnp.float32), sr)
         - jnp.einsum('gnp,blgp->blgn', c_im.astype(jnp.float32), si))
    y = y.reshape(bsz, seq, S5_WIDTH) + d_skip.astype(jnp.float32) * uf
    z = jax.nn.gelu(y, approximate=False)
    z = z * jax.nn.sigmoid(z @ w_glu.astype(jnp.float32) + b_glu.astype(jnp.float32))
    return z.astype(u.dtype)


def rotary(x, positions):
    half = x.shape[-1] // 2
    inv_freq = 1.0 / (ROPE_BASE ** (jnp.arange(half, dtype=jnp.float32) / half))
    ang = positions.astype(jnp.float32)[..., None] * inv_freq
    cos = jnp.cos(ang)[:, :, None, :]
    sin = jnp.sin(ang)[:, :, None, :]
    xf = x.astype(jnp.float32)
    x1, x2 = xf[..., :half], xf[..., half:]
    return jnp.concatenate([x1 * cos - x2 * sin, x2 * cos + x1 * sin], axis=-1)


def retention(q, k, v, gate, positions, out_gain):
    bsz, seq, _ = q.shape
    nc = seq // RET_CHUNK
    qh = rotary(q.reshape(bsz, seq, RET_HEADS, RET_DK), positions)
    kh = rotary(k.reshape(bsz, seq, RET_HEADS, RET_DK), positions) * (RET_DK ** -0.5)
    vh = v.astype(jnp.float32).reshape(bsz, seq, RET_HEADS, RET_DV)
    qc = qh.reshape(bsz, nc, RET_CHUNK, RET_HEADS, RET_DK)
    kc = kh.reshape(bsz, nc, RET_CHUNK, RET_HEADS, RET_DK)
    vc = vh.reshape(bsz, nc, RET_CHUNK, RET_HEADS, RET_DV)
    lg = jnp.log1p(-jnp.exp2(-5.0 - jnp.arange(RET_HEADS, dtype=jnp.float32)))
    idx = jnp.arange(RET_CHUNK, dtype=jnp.float32)
    diff = idx[:, None] - idx[None, :]
    dmask = jnp.where(diff[None] >= 0,
                      jnp.exp(jnp.maximum(diff, 0.0)[None] * lg[:, None, None]), 0.0)
    scores = jnp.einsum('bnchd,bnshd->bnhcs', qc, kc) * dmask
    inner = jnp.einsum('bnhcs,bnshe->bnche', scores, vc)
    zeta = jnp.exp((RET_CHUNK - 1.0 - idx)[:, None] * lg[None, :])
    kv = jnp.einsum('bnshd,bnshe,sh->nbhde', kc, vc, zeta)
    g_chunk = jnp.exp(RET_CHUNK * lg)[None, :, None, None]

    def chunk_step(state, kv_j):
        return g_chunk * state + kv_j, state

    init = jnp.zeros((bsz, RET_HEADS, RET_DK, RET_DV), jnp.float32)
    _, r_prev = lax.scan(chunk_step, init, kv)
    xi = jnp.exp((idx + 1.0)[:, None] * lg[None, :])
    cross = jnp.einsum('bnchd,nbhde->bnche', qc, r_prev) * xi[None, None, :, :, None]
    o = (inner + cross).reshape(bsz, seq, RET_HEADS, RET_DV)
    mu = jnp.mean(o, axis=-1, keepdims=True)
    var = jnp.mean(jnp.square(o - mu), axis=-1, keepdims=True)
    o = (o - mu) * lax.rsqrt(var + EPS) * out_gain.astype(jnp.float32)
    o = o.reshape(bsz, seq, RET_WIDTH) * jax.nn.silu(gate.astype(jnp.float32))
    return o.astype(q.dtype)


def memory_cross_attention(h, m, wq, wk, wv, wo):
    bsz, seq, _ = h.shape
    q = (h @ wq).reshape(bsz, seq, X_HEADS, X_HEAD_DIM)
    k = (m @ wk).reshape(bsz, m.shape[1], X_HEADS, X_HEAD_DIM)
    v = (m @ wv).reshape(bsz, m.shape[1], X_HEADS, X_HEAD_DIM)
    s = jnp.einsum('blhd,bmhd->bhlm', q.astype(jnp.float32), k.astype(jnp.float32)) * (X_HEAD_DIM ** -0.5)
    p = jax.nn.softmax(s, axis=-1)
    o = jnp.einsum('bhlm,bmhd->blhd', p, v.astype(jnp.float32)).astype(h.dtype)
    return o.reshape(bsz, seq, D_MODEL) @ wo


def swiglu(h, w_gate, w_up, w_down):
    return (jax.nn.silu(h @ w_gate) * (h @ w_up)) @ w_down


def setup_inputs(seed: int = 0) -> dict:
    key = jax.random.key(seed)
    ks = iter(jax.random.split(key, 40))

    def nrm(shape, scale):
        return jax.random.normal(next(ks), shape, jnp.float32) * scale

    def gain(shape):
        return 1.0 + nrm(shape, 0.02)

    res_scale = 1.0 / math.sqrt(2.0 * DEPTH)
    x = nrm((BATCH, SEQ, D_MODEL), 1.0)
    mem = nrm((BATCH, MEM_LEN, D_MODEL), 1.0)
    positions = jnp.broadcast_to(jnp.arange(SEQ, dtype=jnp.int32)[None, :], (BATCH, SEQ))
    n_idx = jnp.arange(S5_STATE, dtype=jnp.float32)
    lam_re = -0.5 + nrm((DEPTH, S5_GROUPS, S5_STATE), 0.01)
    lam_im = math.pi * n_idx[None, None, :] + nrm((DEPTH, S5_GROUPS, S5_STATE), 0.01)
    log_step = jax.random.uniform(next(ks), (DEPTH, S5_GROUPS), jnp.float32,
                                  math.log(1e-3), math.log(1e-1))
    return {
        "x": x,
        "mem": mem,
        "positions": positions,
        "norm_mix": gain((DEPTH, D_MODEL)),
        "w_in": nrm((DEPTH, D_MODEL, IN_WIDTH), D_MODEL ** -0.5),
        "s5_lambda_re": lam_re,
        "s5_lambda_im": lam_im,
        "s5_log_step": log_step,
        "s5_b_re": nrm((DEPTH, S5_GROUPS, S5_STATE, S5_GROUP_CH), (2 * S5_GROUP_CH) ** -0.5),
        "s5_b_im": nrm((DEPTH, S5_GROUPS, S5_STATE, S5_GROUP_CH), (2 * S5_GROUP_CH) ** -0.5),
        "s5_c_re": nrm((DEPTH, S5_GROUPS, S5_GROUP_CH, S5_STATE), (2 * S5_STATE) ** -0.5),
        "s5_c_im": nrm((DEPTH, S5_GROUPS, S5_GROUP_CH, S5_STATE), (2 * S5_STATE) ** -0.5),
        "s5_d": nrm((DEPTH, S5_WIDTH), 1.0),
        "s5_w_glu": nrm((DEPTH, S5_WIDTH, S5_WIDTH), S5_WIDTH ** -0.5),
        "s5_b_glu": nrm((DEPTH, S5_WIDTH), 0.01),
        "s5_out_norm": gain((DEPTH, S5_WIDTH)),
        "ret_out_norm": gain((DEPTH, RET_HEADS, RET_DV)),
        "w_out": nrm((DEPTH, MIX_WIDTH, D_MODEL), MIX_WIDTH ** -0.5 * res_scale),
        "norm_cross": gain((DEPTH, D_MODEL)),
        "norm_mem": gain((DEPTH, D_MODEL)),
        "w_cq": nrm((DEPTH, D_MODEL, D_MODEL), D_MODEL ** -0.5),
        "w_ck": nrm((DEPTH, D_MODEL, D_MODEL), D_MODEL ** -0.5),
        "w_cv": nrm((DEPTH, D_MODEL, D_MODEL), D_MODEL ** -0.5),
        "w_co": nrm((DEPTH, D_MODEL, D_MODEL), D_MODEL ** -0.5 * res_scale),
        "norm_ffn": gain((DEPTH, D_MODEL)),
        "w_gate": nrm((DEPTH, D_MODEL, D_FF), D_MODEL ** -0.5),
        "w_up": nrm((DEPTH, D_MODEL, D_FF), D_MODEL ** -0.5),
        "w_down": nrm((DEPTH, D_FF, D_MODEL), D_FF ** -0.5 * res_scale),
        "norm_final": gain((D_MODEL,)),
    }


def reference(x, mem, positions, norm_mix, w_in, s5_lambda_re, s5_lambda_im, s5_log_step,
              s5_b_re, s5_b_im, s5_c_re, s5_c_im, s5_d, s5_w_glu, s5_b_glu, s5_out_norm,
              ret_out_norm, w_out, norm_cross, norm_mem, w_cq, w_ck, w_cv, w_co,
              norm_ffn, w_gate, w_up, w_down, norm_final):
    split_at = [S5_WIDTH, S5_WIDTH + RET_WIDTH, S5_WIDTH + 2 * RET_WIDTH, S5_WIDTH + 3 * RET_WIDTH]
    for l in range(DEPTH):
        h = rms_norm(x, norm_mix[l])
        proj = h @ w_in[l]
        u, q, k, v, g = jnp.split(proj, split_at, axis=-1)
        y_ssm = s5_mixer(u, s5_lambda_re[l], s5_lambda_im[l], s5_log_step[l],
                         s5_b_re[l], s5_b_im[l], s5_c_re[l], s5_c_im[l],
                         s5_d[l], s5_w_glu[l], s5_b_glu[l])
        y_ssm = rms_norm(y_ssm, s5_out_norm[l])
        y_ret = retention(q, k, v, g, positions, ret_out_norm[l])
        x = x + jnp.concatenate([y_ssm, y_ret], axis=-1) @ w_out[l]
        h = rms_norm(x, norm_cross[l])
        m = rms_norm(mem, norm_mem[l])
        x = x + memory_cross_attention(h, m, w_cq[l], w_ck[l], w_cv[l], w_co[l])
        h = rms_norm(x, norm_ffn[l])
        x = x + swiglu(h, w_gate[l], w_up[l], w_down[l])
    return rms_norm(x, norm_final)
```

```python
import math
from contextlib import ExitStack
import numpy as np
import concourse.bass as bass
import concourse.mybir as mybir
from concourse.bass_utils import run_bass_kernel_spmd
from concourse.alu_op_type import AluOpType as ALU

F32 = mybir.dt.float32
BF16 = mybir.dt.bfloat16
I32 = mybir.dt.int32
U8 = mybir.dt.uint8
AF = mybir.ActivationFunctionType
AX = mybir.AxisListType

D = 1024
TT = 512
NST = 4
NB = 64
DFF = 2816
NF = 22
EPS = 1e-6
SAME_RAW = True
ENG = ["pe", "act", "dve", "pool", "sp"]
TWO_PI = 2.0 * math.pi
CW1 = 6.28125
CW2 = TWO_PI - 6.28125
PI_SAFE = 3.1415925


class Tok:
    __slots__ = ("name", "w", "r")

    def __init__(self, name):
        self.name = name
        self.w = {}
        self.r = {}


class Sched:
    def __init__(self, nc, stack):
        self.nc = nc
        self.stack = stack
        self.prog = {e: [] for e in ENG}
        self.cnt = {e: 0 for e in ENG}
        self.seen = {e: {} for e in ENG}
        self.sems = {}
        self.dcnt = {}
        for e in ENG:
            self.sems["e:" + e] = stack.enter_context(nc.semaphore("s_" + e))

    def _need(self, eng, key, val, same_ok):
        if key == "e:" + eng and not same_ok:
            return
        if self.seen[eng].get(key, 0) >= val:
            return
        self.seen[eng][key] = val
        self.prog[eng].append(("wait", key, val))

    def _deps(self, eng, reads, writes):
        for t in reads:
            for k, v in t.w.items():
                self._need(eng, k, v, SAME_RAW)
        for t in writes:
            for k, v in t.w.items():
                self._need(eng, k, v, False)
            for k, v in t.r.items():
                self._need(eng, k, v, False)

    def op(self, eng, fn, reads=(), writes=()):
        self._deps(eng, reads, writes)
        self.cnt[eng] += 1
        c = self.cnt[eng]
        key = "e:" + eng
        self.prog[eng].append(("op", fn, key, 1))
        for t in reads:
            t.r[key] = c
        for t in writes:
            t.w[key] = c

    def dma(self, q, fn, dkey, reads=(), writes=()):
        self._deps(q, reads, writes)
        key = "d:" + dkey
        if key not in self.sems:
            self.sems[key] = self.stack.enter_context(self.nc.semaphore("d_" + dkey))
            self.dcnt[key] = 0
        self.dcnt[key] += 16
        v = self.dcnt[key]
        self.prog[q].append(("op", fn, key, 16))
        for t in reads:
            t.r[key] = v
        for t in writes:
            t.w[key] = v

    def barrier(self):
        for e in ENG:
            for f in ENG:
                if f != e and self.cnt[f] > 0:
                    self._need(e, "e:" + f, self.cnt[f], False)
            for k, v in self.dcnt.items():
                if not k.startswith("d:pc_"):
                    self._need(e, k, v, False)

    def emit(self, block):
        def run(eng_name):
            def body(e):
                for item in self.prog[eng_name]:
                    if item[0] == "wait":
                        e.wait_ge(self.sems[item[1]], item[2])
                    else:
                        inst = item[1](e)
                        inst.then_inc(self.sems[item[2]], item[3])
            return body
        block.tensor(run("pe"))
        block.scalar(run("act"))
        block.vector(run("dve"))
        block.gpsimd(run("pool"))
        block.sync(run("sp"))


def host_consts():
    c = np.zeros((128, 1114), np.float32)
    c[:, 0:128] = np.eye(128, dtype=np.float32)
    s_idx = np.arange(128) // 16
    c[:, 128:256] = (s_idx[None, :] >= s_idx[:, None]).astype(np.float32)
    idx = np.arange(128, dtype=np.float64)
    for h in range(4):
        lg = math.log1p(-2.0 ** (-5.0 - h))
        m = np.where(idx[:, None] <= idx[None, :], np.exp(-(idx[:, None] + 1.0) * lg), 0.0) * (128.0 ** -0.5)
        c[:, 256 + h * 128:256 + (h + 1) * 128] = m.astype(np.float32)
        c[:, 768 + h] = np.exp((idx + 1.0) * lg)
        c[:, 772 + h] = np.exp((127.0 - idx) * lg) * (128.0 ** -0.5)
    i = np.arange(128) % 64
    c[:, 776] = 1.0 / (10000.0 ** (i.astype(np.float64) / 64.0))
    c[:, 777] = np.where(np.arange(128) < 64, -1.0, 1.0)
    c[:, 778:842] = 8.0 * np.arange(1, 65, dtype=np.float32)[None, :]
    c[:, 842:858] = np.arange(-7, 9, dtype=np.float32)[None, :]
    c[:, 858:986] = 1.0
    for h in range(16):
        c[h, 986 + np.arange(8) * 16 + h] = 1.0
    return c


GAMMA128 = [math.exp(128.0 * math.log1p(-2.0 ** (-5.0 - h))) for h in range(4)]


def build_program(NT, NL, dbg=False):
    nc = bass.Bass("TRN2", target_bir_lowering=False)
    T = NT * TT
    dr = {}

    def din(name, shape, dt=F32):
        dr[name] = nc.dram_tensor(name, list(shape), dt, kind="ExternalInput").ap()
        return dr[name]

    x_d = din("x", [T, D])
    pos_d = din("pos", [1, T], I32)
    mem_d = din("mem", [256, D])
    nmix_d = din("norm_mix", [NL, D]); ncross_d = din("norm_cross", [NL, D])
    nmem_d = din("norm_mem", [NL, D]); nffn_d = din("norm_ffn", [NL, D]); nfin_d = din("norm_final", [1, D])
    win_d = din("w_in", [NL, D, 3584])
    lre_d = din("lam_re", [NL, 32, 64]); lim_d = din("lam_im", [NL, 32, 64]); lst_d = din("log_step", [NL, 32])
    bre_d = din("b_re", [NL, 32, 64, 16]); bim_d = din("b_im", [NL, 32, 64, 16])
    cre_d = din("c_re", [NL, 32, 16, 64]); cim_d = din("c_im", [NL, 32, 16, 64])
    sd_d = din("s5_d", [NL, 512]); wglu_d = din("w_glu", [NL, 512, 512]); bglu_d = din("b_glu", [NL, 512])
    s5n_d = din("s5_out_norm", [NL, 512]); rn_d = din("ret_out_norm", [NL, 512])
    wout_d = din("w_out", [NL, D, D]); wcq_d = din("w_cq", [NL, D, D]); wck_d = din("w_ck", [NL, D, D])
    wcv_d = din("w_cv", [NL, D, D]); wco_d = din("w_co", [NL, D, D])
    wg_d = din("w_gate", [NL, D, DFF]); wu_d = din("w_up", [NL, D, DFF]); wd_d = din("w_down", [NL, DFF, D])
    cst_d = din("cst", [128, 1114])
    out_d = nc.dram_tensor("out", [T, D], F32, kind="ExternalOutput").ap()
    xs_d = nc.dram_tensor("xscr", [T, D], F32, kind="Internal").ap()
    rot_d = nc.dram_tensor("rotscr", [NT, 2, 128, TT], F32, kind="Internal").ap()
    wsrc = {"in": win_d, "out": wout_d, "cq": wcq_d, "ck": wck_d, "cv": wcv_d, "co": wco_d, "g": wg_d, "u": wu_d, "d": wd_d, "glu": wglu_d}
    wchk = {"in": (7, 8, 512), "out": (2, 8, 512), "cq": (2, 8, 512), "ck": (2, 8, 512), "cv": (2, 8, 512), "co": (2, 8, 512),
            "g": (11, 8, 256), "u": (11, 8, 256), "d": (4, NF, 256), "glu": (1, 4, 512)}
    wbf = {k: nc.dram_tensor("wbf_" + k, [NL, wchk[k][0], 128, wchk[k][1], wchk[k][2]], BF16, kind="Internal").ap() for k in wsrc}
    tW = {(k, l): Tok("w_%s_%d" % (k, l)) for k in wsrc for l in range(NL)}

    stack = ExitStack()
    with stack:
        arena = stack.enter_context(nc.sbuf_tensor("arena", [128, 206 * 1024], U8))
        S = Sched(nc, stack)
        off = [0]

        def alloc(shape, dt, at=None):
            esz = 4 if dt in (F32, I32) else 2
            n = int(np.prod(shape[1:]))
            nbytes = n * esz
            o = off[0] if at is None else at
            if at is None:
                off[0] = (off[0] + nbytes + 63) // 64 * 64
            assert o + nbytes <= 206 * 1024, (o, nbytes)
            v = arena[0:shape[0], o:o + nbytes].bitcast(dt)
            if len(shape) == 3:
                v = v.rearrange("p (a b) -> p a b", a=shape[1])
            elif len(shape) == 4:
                v = v.rearrange("p (a b c) -> p a b c", a=shape[1], b=shape[2])
            return v

        CST = alloc([128, 1114], F32); tCST = Tok("cst")
        IDB = alloc([128, 128], BF16); ONESB = alloc([128, 128], BF16)
        GT3 = [alloc([128, D], F32) for _ in range(3)]; tGT3 = [Tok("gt%d" % i) for i in range(3)]
        GRET = alloc([128, 512], F32); GS5 = alloc([128, 4], F32); BGLU = alloc([128, 4], F32); tGS = Tok("gs")
        WGLU = alloc([128, 4, 512], BF16); tWGLU = Tok("wglu")
        G5 = alloc([128, 32, 128], BF16); WPRE = alloc([128, 16, 128], BF16); WPIM = alloc([128, 16, 128], BF16)
        VRE = alloc([128, 16, 128], BF16); VIM = alloc([128, 16, 128], BF16)
        COSR = alloc([128, 16, 64], F32); SINR = alloc([128, 16, 64], F32); RHO8 = alloc([128, 16], F32)
        tS5C = Tok("s5c")
        STRE = alloc([128, 16], F32); STIM = alloc([128, 16], F32); tST = Tok("s5state")
        RST = alloc([128, 4, 128], F32); RBF = alloc([128, 4, 128], BF16); tR = Tok("rstate")
        KM = alloc([128, 8, 256], BF16); VM = alloc([128, 2, D], BF16); tKV = Tok("memkv")
        SS = alloc([128, 8], F32); tSS = Tok("ss")
        SMALL = alloc([128, 64], F32); tSMALL = Tok("small")
        MVa = [alloc([128, 8], F32) for _ in range(2)]; RSa = [alloc([128, 4], F32) for _ in range(2)]
        wb_off = off[0]
        WB = [alloc([128, 8, 512], BF16) for _ in range(3)]; tWB = [Tok("wb%d" % i) for i in range(3)]
        G128T = alloc([128, 4, 128], F32)
        GFIN = alloc([128, D], F32); tGFIN = Tok("gfin")
        tr0 = off[0]
        XS = [alloc([128, NST, D], F32) for _ in range(2)]; tXS = [[Tok("x%d_%d" % (b, i)) for i in range(NST)] for b in range(2)]
        XT = alloc([128, 8, TT], BF16); tXT = Tok("xt")
        COS2 = alloc([128, TT], F32); SIN2 = alloc([128, TT], F32); tROT = Tok("rot")
        U8B = alloc([128, 32, 64], BF16); tU8 = Tok("u8")
        ZT = alloc([128, 4, TT], BF16); tZT = Tok("zt")
        SPRE = alloc([128, 16, 64], BF16); SPIM = alloc([128, 16, 64], BF16); tSP = Tok("sp")
        qt_off = off[0]
        QT = alloc([128, 4, TT], BF16); KT = alloc([128, 4, TT], BF16); tQT = Tok("qt"); tKT = Tok("kt")
        VV = alloc([128, NST, 512], BF16); tVV = Tok("vv")
        GG = alloc([128, NST, 512], BF16); tGG = Tok("gg")
        YT = alloc([128, 8, TT], BF16); tYT = Tok("yt")
        WD = [alloc([128, NF, 256], BF16, at=qt_off + i * 11264) for i in range(2)] + [alloc([128, NF, 256], BF16, at=wb_off + i * 11264) for i in range(2)]
        tWDs = [(tQT, tKT, tVV), (tVV, tGG, tYT), (tWB[0], tWB[1]), (tWB[1], tWB[2])]
        assert wb_off + 2 * 11264 <= wb_off + 3 * 8192
        assert qt_off + 2 * 11264 <= off[0]
        scr0 = off[0]
        TMUZ = alloc([128, 8, 512], BF16); tTM = Tok("tmuz")
        SC = [alloc([128, 1024], F32) for _ in range(4)] + [alloc([128, 512], F32) for _ in range(2)]
        tSC = [Tok("sc%d" % i) for i in range(6)]
        scr1 = off[0]
        tSC3b = Tok("sc3b"); tSC4b = Tok("sc4b")
        TMUv = TMUZ.rearrange("p t c -> p (t c)").rearrange("p (g t h) -> p g t h", g=32, t=8)
        XN = alloc([128, NST, D], BF16, at=scr0); tXN = [tTM] * NST
        JUNK = SC[5].bitcast(BF16)
        H = alloc([128, NF, TT], BF16, at=scr0); tH = [tTM] + tSC
        QC = alloc([128, 8, TT], BF16, at=scr0)
        OT = alloc([128, 8, TT], BF16, at=scr0 + 8192)
        EX = alloc([128, 1024], F32, at=scr0 + 16384)
        PN = alloc([128, 1024], BF16, at=scr0 + 20480)
        PNT = alloc([128, 8, 128], BF16, at=scr0 + 24576)
        tQC = [tTM]; tOT = [tSC[0], tSC[1]]; tEX = [tSC[2]]; tPN = [tSC[3]]; tPNT = [tSC[4], tSC4b]
        PN2 = [alloc([128, 1024], BF16, at=scr0 + 20480 + i * 2048) for i in range(2)]; tPN2 = [tSC[3], tSC3b]
        print("SBUF used bytes/partition:", off[0])

        PS = [stack.enter_context(nc.psum_tensor("ps%d" % i, [128, 512], F32)) for i in range(6)]
        tPS = [Tok("ps%d" % i) for i in range(6)]
        PTB = [stack.enter_context(nc.psum_tensor("pt%d" % i, [128, 1024], BF16)) for i in range(2)]
        tPT = [Tok("pt%d" % i) for i in range(2)]
        rr = [0, 0]

        def ps():
            i = rr[0] % 6; rr[0] += 1
            return PS[i], tPS[i]

        def pst():
            i = rr[1] % 2; rr[1] += 1
            return PTB[i], tPT[i]

        wrr = [0]

        def wbuf():
            i = wrr[0] % 3; wrr[0] += 1
            wkey[id(tWB[i])] = "wb%d" % i
            return WB[i], tWB[i]

        wkey = {}

        def precast_list(l):
            lst = []
            for k in ("ck", "cv", "glu", "in", "out", "cq", "co", "g", "u", "d"):
                nch, nkt, ncol = wchk[k]
                for c in range(nch):
                    def f(k=k, c=c, ncol=ncol):
                        S.dma("pool", lambda e: e.dma_start(out=wbf[k][l, c], in_=wsrc[k][l][:, c * ncol:(c + 1) * ncol].rearrange("(kt p) n -> p kt n", p=128)),
                              "pc_" + k, (), (tW[(k, l)],))
                    lst.append(f)
            return lst

        def precast(l):
            for f in precast_list(l):
                f()

        evr = [0]

        def ev_eng():
            evr[0] += 1
            return "act" if evr[0] % 2 == 0 else "dve"

        def copy(eng, out, in_, reads, writes):
            if eng == "act":
                S.op("act", lambda e: e.activation(out=out, in_=in_, func=AF.Copy), reads, writes)
            else:
                S.op("dve", lambda e: e.tensor_copy(out=out, in_=in_), reads, writes)

        def wload(buf, tok, name, l, c0, nkt, ncols, key=None):
            assert ncols == wchk[name][2] and c0 % ncols == 0
            src_ap = wbf[name][l, c0 // ncols]
            S.dma("sp", lambda e: e.dma_start(out=buf[:, 0:nkt, 0:ncols], in_=src_ap),
                  wkey[id(tok)], (tW[(name, l)],), (tok,))

        S.dma("pool", lambda e: e.dma_start(out=CST, in_=cst_d[:, :]), "cst", (), (tCST,))
        S.op("dve", lambda e: e.tensor_copy(out=IDB, in_=CST[:, 0:128]), (tCST,), (tCST,))
        S.op("dve", lambda e: e.tensor_copy(out=ONESB, in_=CST[:, 858:986]), (tCST,), (tCST,))
        IDF = CST[:, 0:128]
        for h in range(4):
            S.op("dve", lambda e, h=h: e.memset(G128T[:, h, :], GAMMA128[h]), (), (tCST,))

        def range_reduce(ang, tmpi, tmpf, n, toks):
            S.op("dve", lambda e: e.tensor_scalar(out=tmpi, in0=ang, scalar1=1.0 / TWO_PI, scalar2=None, op0=ALU.mult), toks, toks)
            S.op("dve", lambda e: e.tensor_copy(out=tmpf, in_=tmpi), toks, toks)
            S.op("dve", lambda e: e.scalar_tensor_tensor(out=ang, in0=tmpf, scalar=-CW1, in1=ang, op0=ALU.mult, op1=ALU.add), toks, toks)
            S.op("dve", lambda e: e.scalar_tensor_tensor(out=ang, in0=tmpf, scalar=-CW2, in1=ang, op0=ALU.mult, op1=ALU.add), toks, toks)
            S.op("dve", lambda e: e.tensor_scalar(out=ang, in0=ang, scalar1=PI_SAFE, scalar2=-PI_SAFE, op0=ALU.min, op1=ALU.max), toks, toks)

        def load_gain(row_ap, gi=0):
            S.dma("pool", lambda e: e.dma_start(out=GT3[gi], in_=row_ap.partition_broadcast(128)), "gt%d" % gi, (), (tGT3[gi],))

        def norm_transpose(gi, X, tX):
            gtab, tg = GT3[gi], tGT3[gi]
            for st in range(NST):
                S.op("act", lambda e, st=st: e.activation(out=JUNK, in_=X[:, st, :], func=AF.Square, accum_out=SS[:, st:st + 1]),
                     (tX[st],), (tSC[5], tSS))
            S.op("dve", lambda e: e.tensor_scalar(out=SS[:, 4:8], in0=SS[:, 0:4], scalar1=1.0 / D, scalar2=EPS, op0=ALU.mult, op1=ALU.add), (tSS,), (tSS,))
            S.op("act", lambda e: e.activation(out=SS[:, 4:8], in_=SS[:, 4:8], func=AF.Sqrt), (tSS,), (tSS,))
            S.op("dve", lambda e: e.reciprocal(out=SS[:, 4:8], in_=SS[:, 4:8]), (tSS,), (tSS,))
            for st in range(NST):
                S.op("dve", lambda e, st=st: e.scalar_tensor_tensor(out=XN[:, st, :], in0=X[:, st, :], scalar=SS[:, 4 + st:5 + st], in1=gtab,
                                                                      op0=ALU.mult, op1=ALU.mult), (tX[st], tSS, tg), (tXN[st],))
            for st in range(NST):
                pt, tpt = pst()
                for kt in range(8):
                    S.op("pe", lambda e, st=st, kt=kt, pt=pt: e.transpose(out=pt[:, kt * 128:(kt + 1) * 128], in_=XN[:, st, kt * 128:(kt + 1) * 128], identity=IDB),
                         (tXN[st], tCST), (tpt,))
                copy(ev_eng(), XT[:, :, st * 128:(st + 1) * 128], pt.rearrange("p (k t) -> p k t", k=8), (tpt,), (tXT,))

        def proj_tm_add(lhs, tl, wname, l, nk, X, tX):
            for cc in range(2):
                wb, twb = wbuf()
                wload(wb, twb, wname, l, cc * 512, nk, 512)
                for st in range(NST):
                    p, tp = ps()
                    for kt in range(nk):
                        S.op("pe", lambda e, st=st, kt=kt, p=p, wb=wb: e.matmul(p[:, :], lhsT=lhs[:, kt, st * 128:(st + 1) * 128], rhs=wb[:, kt, :],
                                                                                  start=(kt == 0), stop=(kt == nk - 1)), tl + (twb,), (tp,))
                    S.op("dve", lambda e, st=st, cc=cc, p=p: e.tensor_tensor(out=X[:, st, cc * 512:(cc + 1) * 512], in0=p[:, :], in1=X[:, st, cc * 512:(cc + 1) * 512], op=ALU.add),
                         (tp, tX[st]), (tX[st],))

        def rot_build(ti):
            r0 = ti * TT
            PI_ = SC[0].bitcast(I32)[:, 0:TT]
            S.dma("sp", lambda e: e.dma_start(out=PI_, in_=pos_d[0:1, r0:r0 + TT].partition_broadcast(128)), "pos", (), (tSC[0],))
            S.op("dve", lambda e: e.tensor_copy(out=SC[1][:, 0:TT], in_=PI_), (tSC[0],), (tSC[1],))
            S.op("dve", lambda e: e.tensor_scalar(out=SIN2, in0=SC[1][:, 0:TT], scalar1=CST[:, 776:777], scalar2=None, op0=ALU.mult), (tSC[1], tCST), (tROT,))
            S.op("dve", lambda e: e.tensor_scalar(out=COS2, in0=SIN2, scalar1=math.pi / 2, scalar2=None, op0=ALU.add), (tROT,), (tROT,))
            for tab in (SIN2, COS2):
                range_reduce(tab, SC[0].bitcast(I32)[:, 0:TT], SC[1][:, 0:TT], 0, (tROT, tSC[0], tSC[1]))
            S.op("act", lambda e: e.activation(out=SIN2, in_=SIN2, func=AF.Sin, scale=CST[:, 777:778]), (tROT, tCST), (tROT,))
            S.op("act", lambda e: e.activation(out=COS2, in_=COS2, func=AF.Sin), (tROT,), (tROT,))


            S.dma("sp", lambda e: e.dma_start(out=rot_d[ti, 0], in_=SIN2), "rot", (tROT,), ())
            S.dma("sp", lambda e: e.dma_start(out=rot_d[ti, 1], in_=COS2), "rot", (tROT,), ())

        precast(0)
        for ti_ in range(NT):
            rot_build(ti_)

        def do_layer(l):
            S.barrier()
            pcq = precast_list(l + 1) if l + 1 < NL else []
            pc_per_tile = (len(pcq) + NT - 1) // NT
            S.dma("pool", lambda e: e.dma_start(out=GRET, in_=rn_d[l:l + 1, :].partition_broadcast(128)), "gs", (), (tGS,))
            S.dma("pool", lambda e: e.dma_start(out=GS5, in_=s5n_d[l, :].rearrange("(c p) -> p c", p=128), allow_slow_non_contiguous=True), "gs", (), (tGS,))
            S.dma("pool", lambda e: e.dma_start(out=BGLU, in_=bglu_d[l, :].rearrange("(c p) -> p c", p=128), allow_slow_non_contiguous=True), "gs", (), (tGS,))
            S.dma("sp", lambda e: e.dma_start(out=WGLU, in_=wbf["glu"][l, 0]), "wglu", (tW[("glu", l)],), (tWGLU,))
            S.op("dve", lambda e: e.memset(STRE, 0.0), (), (tST,))
            S.op("dve", lambda e: e.memset(STIM, 0.0), (), (tST,))
            S.op("dve", lambda e: e.memset(RST, 0.0), (), (tR,))
            S.op("dve", lambda e: e.memset(RBF, 0.0), (), (tR,))

            tS = (tSC[0], tSC[1], tSC[2], tSC[3], tSC[4], tSC[5], tTM)
            MEMX = alloc([128, 2, D], F32, at=scr0 + 8192)
            MEMN = alloc([128, 2, D], BF16, at=scr0 + 16384)
            MT = alloc([128, 8, 256], BF16, at=scr0 + 20480)
            GMEM = GT3[0]
            S.dma("pool", lambda e: e.dma_start(out=MEMX, in_=mem_d.rearrange("(a p) d -> p a d", p=128)), "setup", (), tS)
            load_gain(nmem_d[l:l + 1, :], 0)
            for a in range(2):
                S.op("act", lambda e, a=a: e.activation(out=JUNK, in_=MEMX[:, a, :], func=AF.Square, accum_out=SS[:, a:a + 1]), tS, tS + (tSS,))
            S.op("dve", lambda e: e.tensor_scalar(out=SS[:, 4:6], in0=SS[:, 0:2], scalar1=1.0 / D, scalar2=EPS, op0=ALU.mult, op1=ALU.add), (tSS,), (tSS,))
            S.op("act", lambda e: e.activation(out=SS[:, 4:6], in_=SS[:, 4:6], func=AF.Sqrt), (tSS,), (tSS,))
            S.op("dve", lambda e: e.reciprocal(out=SS[:, 4:6], in_=SS[:, 4:6]), (tSS,), (tSS,))
            for a in range(2):
                S.op("dve", lambda e, a=a: e.scalar_tensor_tensor(out=MEMN[:, a, :], in0=MEMX[:, a, :], scalar=SS[:, 4 + a:5 + a], in1=GMEM, op0=ALU.mult, op1=ALU.mult),
                     tS + (tSS, tGT3[0]), tS)
            for a in range(2):
                pt, tpt = pst()
                for kt in range(8):
                    S.op("pe", lambda e, a=a, kt=kt, pt=pt: e.transpose(out=pt[:, kt * 128:(kt + 1) * 128], in_=MEMN[:, a, kt * 128:(kt + 1) * 128], identity=IDB), tS + (tCST,), (tpt,))
                copy("dve", MT[:, :, a * 128:(a + 1) * 128], pt.rearrange("p (k t) -> p k t", k=8), (tpt,), tS)
            for cc in range(2):
                wb, twb = wbuf()
                wload(wb, twb, "ck", l, cc * 512, 8, 512)
                for c4 in range(4):
                    p, tp = ps()
                    for kt in range(8):
                        S.op("pe", lambda e, kt=kt, c4=c4, p=p, wb=wb: e.matmul(p[:, 0:256], lhsT=wb[:, kt, c4 * 128:(c4 + 1) * 128], rhs=MT[:, kt, :], start=(kt == 0), stop=(kt == 7)),
                             tS + (twb,), (tp,))
                    copy(ev_eng(), KM[:, cc * 4 + c4, :], p[:, 0:256], (tp,), (tKV,))
            for cc in range(2):
                wb, twb = wbuf()
                wload(wb, twb, "cv", l, cc * 512, 8, 512)
                for a in range(2):
                    p, tp = ps()
                    for kt in range(8):
                        S.op("pe", lambda e, kt=kt, a=a, p=p, wb=wb: e.matmul(p[:, :], lhsT=MT[:, kt, a * 128:(a + 1) * 128], rhs=wb[:, kt, :], start=(kt == 0), stop=(kt == 7)),
                             tS + (twb,), (tp,))
                    copy(ev_eng(), VM[:, a, cc * 512:(cc + 1) * 512], p[:, :], (tp,), (tKV,))
            S.barrier()

            so = [tr0]

            def sal(shape, dt):
                esz = 4 if dt in (F32, I32) else 2
                nb = int(np.prod(shape[1:])) * esz
                v = alloc(shape, dt, at=so[0])
                so[0] = (so[0] + nb + 63) // 64 * 64
                assert so[0] <= scr1
                return v
            tQ = (tTM,) + tuple(tSC) + (tXT, tROT, tU8, tZT, tSP, tQT, tKT, tVV, tGG, tYT) + tuple(tXS[0]) + tuple(tXS[1])
            LR = sal([128, 16], F32); LI = sal([128, 16], F32); STP = sal([128, 16], F32)
            LRS = sal([128, 16], F32); TH = sal([128, 16], F32); DEN = sal([128, 16], F32)
            FR = sal([128, 16], F32); FI = sal([128, 16], F32); A1R = sal([128, 16], F32); A1I = sal([128, 16], F32)
            T0 = sal([128, 16], F32); T1 = sal([128, 16], F32)
            APR = sal([128, 16, 16], F32); API = sal([128, 16, 16], F32)
            ANG = sal([128, 16, 16], F32); ANGI = sal([128, 16, 16], I32); ANGF = sal([128, 16, 16], F32); MAGK = sal([128, 16, 16], F32)
            LSB = sal([128, 32], F32)
            BR = sal([128, 16, 16], F32); BI = sal([128, 16, 16], F32); BBR = sal([128, 16, 16], F32); BBI = sal([128, 16, 16], F32)
            TB0 = sal([128, 16, 16], F32); TB1 = sal([128, 16, 16], F32)
            CNR = sal([16, 16, 128], F32); CNI = sal([16, 16, 128], F32)
            CTR = sal([128, 16, 16], F32); CTI = sal([128, 16, 16], F32)
            DM = sal([16, 32], F32); D8 = sal([128, 32], F32)
            W1 = sal([128, 16, 128], F32); W2 = sal([128, 16, 128], F32)
            WTR = sal([128, 16, 128], BF16); WTI = sal([128, 16, 128], BF16)
            XRB = sal([128, 16, 128], BF16); XIB = sal([128, 16, 128], BF16)
            ANGR = sal([128, 16, 64], F32); ANGRI = sal([128, 16, 64], I32); ANGRF = sal([128, 16, 64], F32)

            def ld_qp(dst, src2d):
                S.dma("pool", lambda e: e.dma_start(out=dst, in_=src2d.rearrange("(P r) p -> (r p) P", r=2), allow_slow_non_contiguous=True), "setup", (), tQ)
            ld_qp(LR, lre_d[l]); ld_qp(LI, lim_d[l])
            for r in range(2):
                S.dma("pool", lambda e, r=r: e.dma_start(out=LSB[r * 64:(r + 1) * 64, :], in_=lst_d[l:l + 1, :].partition_broadcast(64)), "setup", (), tQ)
                S.dma("pool", lambda e, r=r: e.dma_start(out=BR[r * 64:(r + 1) * 64], in_=bre_d[l].rearrange("(P r) p h -> r p P h", r=2)[r]), "setup", (), tQ)
                S.dma("pool", lambda e, r=r: e.dma_start(out=BI[r * 64:(r + 1) * 64], in_=bim_d[l].rearrange("(P r) p h -> r p P h", r=2)[r]), "setup", (), tQ)
            S.dma("pool", lambda e: e.dma_start(out=CNR, in_=cre_d[l].rearrange("(P r) n p -> n P r p", r=2)), "setup", (), tQ)
            S.dma("pool", lambda e: e.dma_start(out=CNI, in_=cim_d[l].rearrange("(P r) n p -> n P r p", r=2)), "setup", (), tQ)
            S.dma("pool", lambda e: e.dma_start(out=DM, in_=sd_d[l, :].rearrange("(g h) -> h g", h=16), allow_slow_non_contiguous=True), "setup", (), tQ)

            def dv(fn):
                S.op("dve", fn, tQ + (tCST,), tQ)

            def ac(fn):
                S.op("act", fn, tQ + (tCST,), tQ)
            for r in range(2):
                ac(lambda e, r=r: e.activation(out=STP[r * 64:(r + 1) * 64, :], in_=LSB[r * 64:(r + 1) * 64, r::2], func=AF.Exp))
            dv(lambda e: e.tensor_tensor(out=LRS, in0=LR, in1=STP, op=ALU.mult))
            dv(lambda e: e.tensor_tensor(out=TH, in0=LI, in1=STP, op=ALU.mult))
            KP = CST[:, 842:858]
            dv(lambda e: e.tensor_tensor(out=ANG, in0=TH.unsqueeze(2).broadcast_to([128, 16, 16]), in1=KP.unsqueeze(1).broadcast_to([128, 16, 16]), op=ALU.mult))
            dv(lambda e: e.tensor_tensor(out=MAGK, in0=LRS.unsqueeze(2).broadcast_to([128, 16, 16]), in1=KP.unsqueeze(1).broadcast_to([128, 16, 16]), op=ALU.mult))
            ac(lambda e: e.activation(out=MAGK, in_=MAGK, func=AF.Exp))
            dv(lambda e: e.tensor_scalar(out=ANGF, in0=ANG, scalar1=math.pi / 2, scalar2=None, op0=ALU.add))
            dv(lambda e: e.tensor_copy(out=APR, in_=ANGF))
            range_reduce(APR, ANGI, ANGF, 0, tQ)
            ac(lambda e: e.activation(out=APR, in_=APR, func=AF.Sin))
            dv(lambda e: e.tensor_copy(out=API, in_=ANG))
            range_reduce(API, ANGI, ANGF, 0, tQ)
            ac(lambda e: e.activation(out=API, in_=API, func=AF.Sin))
            dv(lambda e: e.tensor_tensor(out=APR, in0=APR, in1=MAGK, op=ALU.mult))
            dv(lambda e: e.tensor_tensor(out=API, in0=API, in1=MAGK, op=ALU.mult))
            dv(lambda e: e.tensor_copy(out=A1R, in_=APR[:, :, 8]))
            dv(lambda e: e.tensor_copy(out=A1I, in_=API[:, :, 8]))
            dv(lambda e: e.tensor_tensor(out=DEN, in0=LR, in1=LR, op=ALU.mult))
            dv(lambda e: e.tensor_tensor(out=T0, in0=LI, in1=LI, op=ALU.mult))
            dv(lambda e: e.tensor_tensor(out=DEN, in0=DEN, in1=T0, op=ALU.add))
            dv(lambda e: e.reciprocal(out=DEN, in_=DEN))
            dv(lambda e: e.tensor_scalar(out=T0, in0=A1R, scalar1=-1.0, scalar2=None, op0=ALU.add))
            dv(lambda e: e.tensor_tensor(out=FR, in0=T0, in1=LR, op=ALU.mult))
            dv(lambda e: e.tensor_tensor(out=T1, in0=A1I, in1=LI, op=ALU.mult))
            dv(lambda e: e.tensor_tensor(out=FR, in0=FR, in1=T1, op=ALU.add))
            dv(lambda e: e.tensor_tensor(out=FR, in0=FR, in1=DEN, op=ALU.mult))
            dv(lambda e: e.tensor_tensor(out=FI, in0=A1I, in1=LR, op=ALU.mult))
            dv(lambda e: e.tensor_tensor(out=T1, in0=T0, in1=LI, op=ALU.mult))
            dv(lambda e: e.tensor_tensor(out=FI, in0=FI, in1=T1, op=ALU.subtract))
            dv(lambda e: e.tensor_tensor(out=FI, in0=FI, in1=DEN, op=ALU.mult))
            bc = lambda a: a.unsqueeze(2).broadcast_to([128, 16, 16])
            dv(lambda e: e.tensor_tensor(out=TB0, in0=BR, in1=bc(FR), op=ALU.mult))
            dv(lambda e: e.tensor_tensor(out=TB1, in0=BI, in1=bc(FI), op=ALU.mult))
            dv(lambda e: e.tensor_tensor(out=BBR, in0=TB0, in1=TB1, op=ALU.subtract))
            dv(lambda e: e.tensor_tensor(out=TB0, in0=BI, in1=bc(FR), op=ALU.mult))
            dv(lambda e: e.tensor_tensor(out=TB1, in0=BR, in1=bc(FI), op=ALU.mult))
            dv(lambda e: e.tensor_tensor(out=BBI, in0=TB0, in1=TB1, op=ALU.add))
            for (CN, CT_) in ((CNR, CTR), (CNI, CTI)):
                p, tp = ps()
                for P in range(16):
                    S.op("pe", lambda e, P=P, p=p, CN=CN: e.transpose(out=p[:, P * 16:(P + 1) * 16], in_=CN[:, P, :], identity=IDF[0:16, 0:16]), tQ + (tCST,), (tp,))
                S.op("dve", lambda e, p=p, CT_=CT_: e.tensor_copy(out=CT_, in_=p[:, 0:256].rearrange("p (a b) -> p a b", a=16)), (tp,), tQ)
            p, tp = ps()
            S.op("pe", lambda e, p=p: e.matmul(p[:, 0:32], lhsT=CST[0:16, 986:1114], rhs=DM, start=True, stop=True), tQ + (tCST,), (tp,))
            S.op("dve", lambda e, p=p: e.tensor_copy(out=D8, in_=p[:, 0:32]), (tp,), tQ)
            W14 = W1.rearrange("p a (s h) -> p a s h", s=8); W24 = W2.rearrange("p a (s h) -> p a s h", s=8)

            def pw(AP_, lo, rev):
                v = AP_[:, :, lo:lo + 8]
                if rev:
                    v = AP_[:, :, lo + 7:lo - 1 if lo > 0 else None:-1]
                return v.unsqueeze(3).broadcast_to([128, 16, 8, 16])
            b4 = lambda a: a.unsqueeze(2).broadcast_to([128, 16, 8, 16])
            for s in range(8):
                k = 14 - s
                pr = lambda: APR[:, :, k:k + 1].broadcast_to([128, 16, 16])
                pi_ = lambda: API[:, :, k:k + 1].broadcast_to([128, 16, 16])
                dv(lambda e, s=s, k=k: e.tensor_tensor(out=W14[:, :, s, :], in0=BBR, in1=APR[:, :, k:k + 1].broadcast_to([128, 16, 16]), op=ALU.mult))
                dv(lambda e, s=s, k=k: e.tensor_tensor(out=W24[:, :, s, :], in0=BBI, in1=API[:, :, k:k + 1].broadcast_to([128, 16, 16]), op=ALU.mult))
            dv(lambda e: e.tensor_tensor(out=WTR, in0=W1, in1=W2, op=ALU.subtract))
            for s in range(8):
                k = 14 - s
                dv(lambda e, s=s, k=k: e.tensor_tensor(out=W14[:, :, s, :], in0=BBI, in1=APR[:, :, k:k + 1].broadcast_to([128, 16, 16]), op=ALU.mult))
                dv(lambda e, s=s, k=k: e.tensor_tensor(out=W24[:, :, s, :], in0=BBR, in1=API[:, :, k:k + 1].broadcast_to([128, 16, 16]), op=ALU.mult))
            dv(lambda e: e.tensor_tensor(out=WTI, in0=W1, in1=W2, op=ALU.add))
            for (WT_, WP_) in ((WTR, WPRE), (WTI, WPIM)):
                for half in range(2):
                    pt, tpt = pst()
                    for P8 in range(8):
                        P = half * 8 + P8
                        S.op("pe", lambda e, P=P, P8=P8, pt=pt, WT_=WT_: e.transpose(out=pt[:, P8 * 128:(P8 + 1) * 128], in_=WT_[:, P, :], identity=IDB), tQ + (tCST,), (tpt,))
                    S.op("dve", lambda e, half=half, pt=pt, WP_=WP_: e.tensor_copy(out=WP_[:, half * 8:(half + 1) * 8, :], in_=pt.rearrange("p (a b) -> p a b", a=8)), (tpt,), (tS5C,))
            for (dstR, dstI, k0) in ((VRE, VIM, 8), (XRB, XIB, 0)):
                for t in range(8):
                    k = k0 + t
                    dv(lambda e, t=t, k=k: e.tensor_tensor(out=W14[:, :, t, :], in0=CTR, in1=APR[:, :, k:k + 1].broadcast_to([128, 16, 16]), op=ALU.mult))
                    dv(lambda e, t=t, k=k: e.tensor_tensor(out=W24[:, :, t, :], in0=CTI, in1=API[:, :, k:k + 1].broadcast_to([128, 16, 16]), op=ALU.mult))
                S.op("dve", lambda e, dstR=dstR: e.tensor_tensor(out=dstR, in0=W1, in1=W2, op=ALU.subtract), tQ, tQ + (tS5C,))
                for t in range(8):
                    k = k0 + t
                    dv(lambda e, t=t, k=k: e.tensor_tensor(out=W14[:, :, t, :], in0=CTR, in1=API[:, :, k:k + 1].broadcast_to([128, 16, 16]), op=ALU.mult))
                    dv(lambda e, t=t, k=k: e.tensor_tensor(out=W24[:, :, t, :], in0=CTI, in1=APR[:, :, k:k + 1].broadcast_to([128, 16, 16]), op=ALU.mult))
                S.op("dve", lambda e, dstI=dstI: e.scalar_tensor_tensor(out=dstI, in0=W1, scalar=-1.0, in1=W2, op0=ALU.mult, op1=ALU.subtract), tQ, tQ + (tS5C,))
            for g in range(32):
                P, r = g // 2, g % 2
                p, tp = ps()
                S.op("pe", lambda e, P=P, r=r, p=p: e.matmul(p[:, 0:128], lhsT=WTR[r * 64:(r + 1) * 64, P, :], rhs=XRB[r * 64:(r + 1) * 64, P, :], start=True, stop=False), tQ, (tp,))
                S.op("pe", lambda e, P=P, r=r, p=p: e.matmul(p[:, 0:128], lhsT=WTI[r * 64:(r + 1) * 64, P, :], rhs=XIB[r * 64:(r + 1) * 64, P, :], start=False, stop=True), tQ, (tp,))
                S.op("dve", lambda e, p=p: e.tensor_tensor(out=W1[:, 0, :], in0=p[:, 0:128], in1=CST[:, 128:256], op=ALU.mult), (tp, tCST) + tQ, tQ)
                S.op("dve", lambda e, g=g: e.scalar_tensor_tensor(out=G5[:, g, :], in0=CST[:, 0:128], scalar=D8[:, g:g + 1], in1=W1[:, 0, :], op0=ALU.mult, op1=ALU.add),
                     tQ + (tCST,), (tS5C,))
            S.op("act", lambda e: e.activation(out=RHO8, in_=LRS, func=AF.Exp, scale=8.0), tQ, (tS5C,))
            M8 = CST[:, 778:842]
            dv(lambda e: e.tensor_tensor(out=ANGR, in0=TH.unsqueeze(2).broadcast_to([128, 16, 64]), in1=M8.unsqueeze(1).broadcast_to([128, 16, 64]), op=ALU.mult))
            dv(lambda e: e.tensor_scalar(out=ANGRF, in0=ANGR, scalar1=math.pi / 2, scalar2=None, op0=ALU.add))
            S.op("dve", lambda e: e.tensor_copy(out=COSR, in_=ANGRF), tQ, (tS5C,))
            range_reduce(COSR, ANGRI, ANGRF, 0, tQ + (tS5C,))
            S.op("act", lambda e: e.activation(out=COSR, in_=COSR, func=AF.Sin), (tS5C,), (tS5C,))
            S.op("dve", lambda e: e.tensor_copy(out=SINR, in_=ANGR), tQ, (tS5C,))
            range_reduce(SINR, ANGRI, ANGRF, 0, tQ + (tS5C,))
            S.op("act", lambda e: e.activation(out=SINR, in_=SINR, func=AF.Sin), (tS5C,), (tS5C,))
            S.barrier()

            load_gain(nmix_d[l:l + 1, :], 0); load_gain(ncross_d[l:l + 1, :], 1); load_gain(nffn_d[l:l + 1, :], 2)
            pending = []
            def do_tile(ti):
                r0 = ti * TT
                src = x_d if l == 0 else xs_d
                X, tX = XS[ti % 2], tXS[ti % 2]

                def xload(tj):
                    Xj, tXj = XS[tj % 2], tXS[tj % 2]
                    for st in range(NST):
                        S.dma("pool", lambda e, st=st: e.dma_start(out=Xj[:, st, :], in_=src[tj * TT + st * 128:tj * TT + (st + 1) * 128, :]), "x%d_%d" % (tj % 2, st), (), (tXj[st],))

                S.dma("pool", lambda e: e.dma_start(out=SIN2, in_=rot_d[ti, 0]), "rotl", (), (tROT,))
                S.dma("pool", lambda e: e.dma_start(out=COS2, in_=rot_d[ti, 1]), "rotl", (), (tROT,))
                while pending:
                    pending.pop(0)()
                if ti == 0:
                    xload(0)
                if ti + 1 < NT:
                    xload(ti + 1)

                norm_transpose(0, X, tX)
                wb, twb = wbuf()
                wload(wb, twb, "in", l, 0, 8, 512)
                for t2 in range(4):
                    p, tp = ps()
                    for kt in range(8):
                        for r in range(2):
                            t = 2 * t2 + r
                            S.op("pe", lambda e, t=t, r=r, kt=kt, p=p, wb=wb: e.matmul(p[r * 64:(r + 1) * 64, :], lhsT=XT[:, kt, t::8], rhs=wb[:, kt, :], start=(kt == 0), stop=(kt == 7)),
                                 (tXT, twb), (tp,))
                    for r in range(2):
                        t = 2 * t2 + r
                        S.op("dve", lambda e, t=t, r=r, p=p: e.tensor_copy(out=TMUv[0:64, :, t, :], in_=p[r * 64:(r + 1) * 64, :].rearrange("p (g h) -> p g h", g=32)), (tp,), (tTM,))
                for half in range(2):
                    pt, tpt = pst()
                    for g16 in range(16):
                        g = half * 16 + g16
                        S.op("pe", lambda e, g=g, g16=g16, pt=pt: e.transpose(out=pt[:, g16 * 64:(g16 + 1) * 64], in_=TMUv[0:64, g, :, :].rearrange("p t h -> p (t h)"), identity=IDB[0:64, 0:64]),
                             (tTM, tCST), (tpt,))
                    copy(ev_eng(), U8B[:, half * 16:(half + 1) * 16, :], pt.rearrange("p (g j) -> p g j", g=16), (tpt,), (tU8,))
                zb = []
                for half in range(2):
                    zr, tzr = ps()
                    zi, tzi = ps()
                    zb.append((zr, tzr, zi, tzi))
                    for P8 in range(8):
                        P = half * 8 + P8
                        for r in range(2):
                            g = 2 * P + r
                            S.op("pe", lambda e, P=P, P8=P8, r=r, g=g, zr=zr: e.matmul(zr[r * 64:(r + 1) * 64, P8 * 64:(P8 + 1) * 64], lhsT=WPRE[:, P, r * 64:(r + 1) * 64], rhs=U8B[:, g, :], start=True, stop=True),
                                 (tS5C, tU8), (tzr,))
                            S.op("pe", lambda e, P=P, P8=P8, r=r, g=g, zi=zi: e.matmul(zi[r * 64:(r + 1) * 64, P8 * 64:(P8 + 1) * 64], lhsT=WPIM[:, P, r * 64:(r + 1) * 64], rhs=U8B[:, g, :], start=True, stop=True),
                                 (tS5C, tU8), (tzi,))
                tA, tB, tC, tD = tSC[0], tSC[1], tSC[2], tSC[3]

                def halfv(half):
                    hs = slice(half * 8, (half + 1) * 8)
                    cosr = COSR[:, hs, :].rearrange("p a b -> p (a b)")
                    sinr = SINR[:, hs, :].rearrange("p a b -> p (a b)")
                    A_, B_, C_, D_ = (SC[i][:, half * 512:(half + 1) * 512] for i in range(4))
                    return hs, cosr, sinr, A_, B_, C_, D_
                for half in range(2):
                    hs, cosr, sinr, A_, B_, C_, D_ = halfv(half)
                    zr, tzr, zi, tzi = zb[half]
                    S.op("dve", lambda e, zr=zr, cosr=cosr, A_=A_: e.tensor_tensor(out=A_, in0=zr[:, :], in1=cosr, op=ALU.mult), (tzr, tS5C), (tA,))
                    S.op("dve", lambda e, zi=zi, sinr=sinr, B_=B_: e.tensor_tensor(out=B_, in0=zi[:, :], in1=sinr, op=ALU.mult), (tzi, tS5C), (tB,))
                    S.op("dve", lambda e, A_=A_, B_=B_: e.tensor_tensor(out=A_, in0=A_, in1=B_, op=ALU.add), (tA, tB), (tA,))
                    S.op("dve", lambda e, zi=zi, cosr=cosr, C_=C_: e.tensor_tensor(out=C_, in0=zi[:, :], in1=cosr, op=ALU.mult), (tzi, tS5C), (tC,))
                    S.op("dve", lambda e, zr=zr, sinr=sinr, B_=B_: e.tensor_tensor(out=B_, in0=zr[:, :], in1=sinr, op=ALU.mult), (tzr, tS5C, tA), (tB,))
                    S.op("dve", lambda e, C_=C_, B_=B_: e.tensor_tensor(out=C_, in0=C_, in1=B_, op=ALU.subtract), (tC, tB), (tC,))
                for half in range(2):
                    hs, cosr, sinr, A_, B_, C_, D_ = halfv(half)
                    for P8 in range(8):
                        P = half * 8 + P8
                        cs = slice(P8 * 64, (P8 + 1) * 64)
                        S.op("dve", lambda e, P=P, cs=cs, A_=A_, B_=B_: e.tensor_tensor_scan(out=B_[:, cs], data0=RHO8[:, P:P + 1].broadcast_to([128, 64]), data1=A_[:, cs],
                                                                                            initial=STRE[:, P:P + 1], op0=ALU.mult, op1=ALU.add), (tA, tS5C, tST, tC), (tB,))
                        S.op("dve", lambda e, P=P, cs=cs, C_=C_, D_=D_: e.tensor_tensor_scan(out=D_[:, cs], data0=RHO8[:, P:P + 1].broadcast_to([128, 64]), data1=C_[:, cs],
                                                                                            initial=STIM[:, P:P + 1], op0=ALU.mult, op1=ALU.add), (tC, tS5C, tST), (tD,))
                    S.op("dve", lambda e, A_=A_, B_=B_, cosr=cosr: e.tensor_tensor(out=A_, in0=B_, in1=cosr, op=ALU.mult), (tB, tS5C), (tA,))
                    S.op("dve", lambda e, C_=C_, D_=D_, sinr=sinr: e.tensor_tensor(out=C_, in0=D_, in1=sinr, op=ALU.mult), (tD, tS5C), (tC,))
                    S.op("dve", lambda e, A_=A_, C_=C_: e.tensor_tensor(out=A_, in0=A_, in1=C_, op=ALU.subtract), (tA, tC), (tA,))
                    S.op("dve", lambda e, C_=C_, D_=D_, cosr=cosr: e.tensor_tensor(out=C_, in0=D_, in1=cosr, op=ALU.mult), (tD, tS5C, tA), (tC,))
                    S.op("dve", lambda e, B_=B_, sinr=sinr: e.tensor_tensor(out=B_, in0=B_, in1=sinr, op=ALU.mult), (tB, tS5C), (tB,))
                    S.op("dve", lambda e, C_=C_, B_=B_: e.tensor_tensor(out=C_, in0=C_, in1=B_, op=ALU.add), (tC, tB), (tC,))
                    A3 = A_.rearrange("p (a b) -> p a b", a=8); C3 = C_.rearrange("p (a b) -> p a b", a=8)
                    S.op("dve", lambda e, hs=hs: e.tensor_copy(out=SPRE[:, hs, 0:1], in_=STRE[:, hs].unsqueeze(2)), (tST,), (tSP,))
                    S.op("dve", lambda e, hs=hs: e.tensor_copy(out=SPIM[:, hs, 0:1], in_=STIM[:, hs].unsqueeze(2)), (tST,), (tSP,))
                    S.op("dve", lambda e, hs=hs, A3=A3: e.tensor_copy(out=SPRE[:, hs, 1:64], in_=A3[:, :, 0:63]), (tA,), (tSP,))
                    S.op("dve", lambda e, hs=hs, C3=C3: e.tensor_copy(out=SPIM[:, hs, 1:64], in_=C3[:, :, 0:63]), (tC,), (tSP,))
                    S.op("dve", lambda e, hs=hs, A3=A3: e.tensor_copy(out=STRE[:, hs].unsqueeze(2), in_=A3[:, :, 63:64]), (tA, tSP), (tST,))
                    S.op("dve", lambda e, hs=hs, C3=C3: e.tensor_copy(out=STIM[:, hs].unsqueeze(2), in_=C3[:, :, 63:64]), (tC, tSP), (tST,))
                RA, RB_ = SC[4][:, 0:512], SC[5][:, 0:512]
                tRA = (tSC[4], tSC4b); tRBt = (tSC[5],)
                for (ci, cis, DST, tD_) in ((1, 5, QT, tQT), (2, 6, KT, tKT)):
                    wa, twa = wbuf(); wload(wa, twa, "in", l, ci * 512, 8, 512)
                    wsb, twsb = wbuf(); wload(wsb, twsb, "in", l, cis * 512, 8, 512)
                    for h in range(4):
                        p1, tp1 = ps(); p2, tp2 = ps()
                        for kt in range(8):
                            S.op("pe", lambda e, h=h, kt=kt, p1=p1, wa=wa: e.matmul(p1[:, :], lhsT=wa[:, kt, h * 128:(h + 1) * 128], rhs=XT[:, kt, :], start=(kt == 0), stop=(kt == 7)), (twa, tXT), (tp1,))
                        for kt in range(8):
                            S.op("pe", lambda e, h=h, kt=kt, p2=p2, wsb=wsb: e.matmul(p2[:, :], lhsT=wsb[:, kt, h * 128:(h + 1) * 128], rhs=XT[:, kt, :], start=(kt == 0), stop=(kt == 7)), (twsb, tXT), (tp2,))
                        S.op("act", lambda e, p1=p1: e.activation(out=RA, in_=p1[:, :], func=AF.Copy), (tp1,), tRA)
                        S.op("act", lambda e, p2=p2: e.activation(out=RB_, in_=p2[:, :], func=AF.Copy), (tp2,), tRBt)
                        S.op("pool", lambda e: e.tensor_tensor(out=RA, in0=RA, in1=COS2, op=ALU.mult), tRA + (tROT,), tRA)
                        S.op("pool", lambda e: e.tensor_tensor(out=RB_, in0=RB_, in1=SIN2, op=ALU.mult), tRBt + (tROT,), tRBt)
                        S.op("pool", lambda e, h=h, DST=DST: e.tensor_tensor(out=DST[:, h, :], in0=RA, in1=RB_, op=ALU.add), tRA + tRBt, (tD_,))
                for (ci, is_g) in ((3, False), (4, True)):
                    wa, twa = wbuf(); wload(wa, twa, "in", l, ci * 512, 8, 512)
                    for st in range(NST):
                        p, tp = ps()
                        for kt in range(8):
                            S.op("pe", lambda e, st=st, kt=kt, p=p, wa=wa: e.matmul(p[:, :], lhsT=XT[:, kt, st * 128:(st + 1) * 128], rhs=wa[:, kt, :], start=(kt == 0), stop=(kt == 7)), (twa, tXT), (tp,))
                        if not is_g:
                            copy("act", VV[:, st, :], p[:, :], (tp,), (tVV,))
                        else:
                            S.op("act", lambda e, p=p: e.activation(out=RB_, in_=p[:, :], func=AF.Silu), (tp,), tRBt)
                            S.op("pool", lambda e, st=st: e.tensor_tensor(out=GG[:, st, :], in0=RB_, in1=GRET, op=ALU.mult), tRBt + (tGS,), (tGG,))
                for g4 in range(8):
                    p, tp = ps()
                    for gi in range(4):
                        g = g4 * 4 + gi
                        P, r = g // 2, g % 2
                        o = p[0:64, gi * 128:(gi + 1) * 128]
                        S.op("pe", lambda e, g=g, o=o: e.matmul(o, lhsT=U8B[:, g, :], rhs=G5[:, g, :], start=True, stop=False), (tU8, tS5C), (tp,))
                        S.op("pe", lambda e, P=P, r=r, o=o: e.matmul(o, lhsT=SPRE[r * 64:(r + 1) * 64, P, :], rhs=VRE[r * 64:(r + 1) * 64, P, :], start=False, stop=False), (tSP, tS5C), (tp,))
                        S.op("pe", lambda e, P=P, r=r, o=o: e.matmul(o, lhsT=SPIM[r * 64:(r + 1) * 64, P, :], rhs=VIM[r * 64:(r + 1) * 64, P, :], start=False, stop=True), (tSP, tS5C), (tp,))
                    S.op("act", lambda e, g4=g4, p=p: e.activation(out=TMUZ[0:64, :, g4 * 64:(g4 + 1) * 64].rearrange("p t (g n) -> p g t n", g=4),
                                                                    in_=p[0:64, :].rearrange("p (g t n) -> p g t n", g=4, t=8), func=AF.Gelu), (tp, tU8), (tTM,))
                for ct in range(4):
                    pt, tpt = pst()
                    for t in range(8):
                        S.op("pe", lambda e, ct=ct, t=t, pt=pt: e.transpose(out=pt[:, t * 64:(t + 1) * 64], in_=TMUZ[0:64, t, ct * 128:(ct + 1) * 128], identity=IDB[0:64, 0:64]),
                             (tTM, tCST), (tpt,))
                    copy(ev_eng(), ZT[:, ct, :].rearrange("p (j t) -> p j t", t=8), pt[:, 0:512].rearrange("p (t j) -> p j t", t=8), (tpt,), (tZT,))
                ZZ = SC[0].bitcast(BF16)
                SQ = SC[1].bitcast(BF16)
                for ct2 in range(4):
                    p, tp = ps()
                    for ct in range(4):
                        S.op("pe", lambda e, ct=ct, ct2=ct2, p=p: e.matmul(p[:, :], lhsT=WGLU[:, ct, ct2 * 128:(ct2 + 1) * 128], rhs=ZT[:, ct, :], start=(ct == 0), stop=(ct == 3)),
                             (tWGLU, tZT), (tp,))
                    S.op("act", lambda e, ct2=ct2, p=p: e.activation(out=SC[2][:, 0:512], in_=p[:, :], func=AF.Sigmoid, bias=BGLU[:, ct2:ct2 + 1]), (tp, tGS), (tSC[2],))
                    S.op("dve", lambda e, ct2=ct2: e.tensor_tensor(out=ZZ[:, ct2 * 512:(ct2 + 1) * 512], in0=ZT[:, ct2, :], in1=SC[2][:, 0:512], op=ALU.mult), (tZT, tSC[2]), (tSC[0],))
                    S.op("dve", lambda e, ct2=ct2: e.tensor_tensor(out=SQ[:, ct2 * 512:(ct2 + 1) * 512], in0=ZZ[:, ct2 * 512:(ct2 + 1) * 512], in1=ZZ[:, ct2 * 512:(ct2 + 1) * 512], op=ALU.mult),
                         (tSC[0],), (tSC[1],))
                p, tp = ps()
                for ct in range(4):
                    S.op("pe", lambda e, ct=ct, p=p: e.matmul(p[:, :], lhsT=ONESB, rhs=SQ[:, ct * 512:(ct + 1) * 512], start=(ct == 0), stop=(ct == 3)), (tSC[1], tCST), (tp,))
                S.op("act", lambda e, p=p: e.activation(out=SC[3][:, 0:512], in_=p[:, :], func=AF.Sqrt, scale=1.0 / 512, bias=EPS), (tp,), (tSC[3],))
                S.op("dve", lambda e: e.reciprocal(out=SC[3][:, 0:512], in_=SC[3][:, 0:512]), (tSC[3],), (tSC[3],))
                for ct in range(4):
                    S.op("dve", lambda e, ct=ct: e.scalar_tensor_tensor(out=YT[:, ct, :], in0=ZZ[:, ct * 512:(ct + 1) * 512], scalar=GS5[:, ct:ct + 1], in1=SC[3][:, 0:512], op0=ALU.mult, op1=ALU.mult),
                         (tSC[0], tSC[3], tGS), (tYT,))

                KTM4 = SC[0].bitcast(BF16).rearrange("p (s h d) -> p s h d", s=4, h=4); tKTM = tSC[0]
                PT4 = SC[1].bitcast(BF16).rearrange("p (s h d) -> p s h d", s=4, h=4); tPT = tSC[1]
                RB4 = SC[2].bitcast(BF16).rearrange("p (s h d) -> p s h d", s=4, h=4); tRB = tSC[2]
                OO2 = [SC[3][:, i * 512:(i + 1) * 512].rearrange("p (h d) -> p h d", h=4) for i in range(2)]; tOO2 = [tSC[3], tSC3b]
                YR2 = [SC[4].bitcast(BF16)[:, i * 512:(i + 1) * 512] for i in range(2)]; tYR2 = [tSC[4], tSC4b]
                BNa = [SMALL[:, 8 + i * 24:8 + (i + 1) * 24] for i in range(2)]; tBN = tSMALL
                for st in range(NST):
                    cs = slice(st * 128, (st + 1) * 128)
                    pt, tpt = pst()
                    for h in range(4):
                        S.op("pe", lambda e, h=h, cs=cs, pt=pt: e.transpose(out=pt[:, h * 128:(h + 1) * 128], in_=KT[:, h, cs], identity=IDB), (tKT, tCST), (tpt,))
                    for h in range(4):
                        S.op("act", lambda e, h=h, st=st, pt=pt: e.activation(out=KTM4[:, st, h, :], in_=pt[:, h * 128:(h + 1) * 128], func=AF.Copy, scale=CST[:, 772 + h:773 + h]),
                             (tpt, tCST), (tKTM,))
                    p, tp = ps()
                    for h in range(4):
                        S.op("pe", lambda e, h=h, cs=cs, p=p: e.matmul(p[:, h * 128:(h + 1) * 128], lhsT=KT[:, h, cs], rhs=QT[:, h, cs], start=True, stop=True), (tKT, tQT), (tp,))
                    S.op("dve", lambda e, st=st, p=p: e.tensor_tensor(out=PT4[:, st].rearrange("p h d -> p (h d)"), in0=p[:, :], in1=CST[:, 256:768], op=ALU.mult), (tp, tCST), (tPT,))
                    pk, tpk = ps()
                    for h in range(4):
                        S.op("pe", lambda e, h=h, st=st, pk=pk: e.matmul(pk[:, h * 128:(h + 1) * 128], lhsT=KTM4[:, st, h, :], rhs=VV[:, st, h * 128:(h + 1) * 128], start=True, stop=True), (tKTM, tVV), (tpk,))
                    S.op("dve", lambda e, st=st: e.tensor_copy(out=RB4[:, st], in_=RST), (tR,), (tRB,))
                    S.op("dve", lambda e: e.tensor_tensor(out=RST, in0=RST, in1=G128T, op=ALU.mult), (tR, tRB, tCST), (tR,))
                    S.op("dve", lambda e, pk=pk: e.tensor_tensor(out=RST.rearrange("p h d -> p (h d)"), in0=pk[:, :], in1=RST.rearrange("p h d -> p (h d)"), op=ALU.add), (tpk, tR), (tR,))

                def ret_o(st):
                    cs = slice(st * 128, (st + 1) * 128)
                    OO, tOO = OO2[st % 2], tOO2[st % 2]
                    YR, tYR = YR2[st % 2], tYR2[st % 2]
                    BN = BNa[st % 2]
                    po, tpo = ps()
                    for h in range(4):
                        S.op("pe", lambda e, h=h, po=po: e.matmul(po[:, h * 128:(h + 1) * 128], lhsT=PT4[:, st, h, :], rhs=VV[:, st, h * 128:(h + 1) * 128], start=True, stop=False), (tPT, tVV), (tpo,))
                        S.op("pe", lambda e, h=h, po=po: e.matmul(po[:, h * 128:(h + 1) * 128], lhsT=QT[:, h, cs], rhs=RB4[:, st, h, :], start=False, stop=True), (tQT, tRB), (tpo,))
                    for h in range(4):
                        S.op("act", lambda e, h=h, po=po: e.activation(out=OO[:, h, :], in_=po[:, h * 128:(h + 1) * 128], func=AF.Copy, scale=CST[:, 768 + h:769 + h]), (tpo, tCST), (tOO,))
                    OOf = OO.rearrange("p h d -> p (h d)")
                    SQt = SC[5][:, 0:512]
                    MV = MVa[st % 2]; RS_ = RSa[st % 2]
                    S1 = MV[:, 0:4]; S2 = MV[:, 4:8]
                    S.op("dve", lambda e: e.tensor_reduce(out=S1, in_=OO, axis=AX.X, op=ALU.add), (tOO,), (tBN,))
                    S.op("dve", lambda e: e.tensor_tensor(out=SQt, in0=OOf, in1=OOf, op=ALU.mult), (tOO,), (tSC[5],))
                    S.op("dve", lambda e: e.tensor_reduce(out=S2, in_=SQt.rearrange("p (h d) -> p h d", h=4), axis=AX.X, op=ALU.add), (tSC[5],), (tBN,))
                    S.op("dve", lambda e: e.tensor_scalar(out=S1, in0=S1, scalar1=1.0 / 128.0, scalar2=None, op0=ALU.mult), (tBN,), (tBN,))
                    S.op("dve", lambda e: e.tensor_tensor(out=RS_, in0=S1, in1=S1, op=ALU.mult), (tBN,), (tBN,))
                    S.op("dve", lambda e: e.scalar_tensor_tensor(out=RS_, in0=S2, scalar=1.0 / 128.0, in1=RS_, op0=ALU.mult, op1=ALU.subtract), (tBN,), (tBN,))
                    S.op("act", lambda e: e.activation(out=RS_, in_=RS_, func=AF.Sqrt, bias=EPS), (tBN,), (tBN,))
                    S.op("dve", lambda e: e.reciprocal(out=RS_, in_=RS_), (tBN,), (tBN,))
                    S.op("dve", lambda e: e.tensor_tensor(out=OO, in0=OO, in1=S1.unsqueeze(2).broadcast_to([128, 4, 128]), op=ALU.subtract), (tOO, tBN), (tOO,))
                    S.op("dve", lambda e: e.tensor_tensor(out=OO, in0=OO, in1=RS_.unsqueeze(2).broadcast_to([128, 4, 128]), op=ALU.mult), (tOO, tBN), (tOO,))
                    S.op("dve", lambda e: e.tensor_tensor(out=YR, in0=OOf, in1=GG[:, st, :], op=ALU.mult), (tOO, tGG), (tYR,))

                def ret_t(st):
                    cs = slice(st * 128, (st + 1) * 128)
                    YR, tYR = YR2[st % 2], tYR2[st % 2]
                    pt, tpt = pst()
                    for h in range(4):
                        S.op("pe", lambda e, h=h, pt=pt: e.transpose(out=pt[:, h * 128:(h + 1) * 128], in_=YR[:, h * 128:(h + 1) * 128], identity=IDB), (tYR, tCST), (tpt,))
                    copy("act", YT[:, 4:8, cs], pt[:, 0:512].rearrange("p (h t) -> p h t", h=4), (tpt,), (tYT,))
                ret_o(0)
                for st in range(1, NST):
                    ret_o(st)
                    ret_t(st - 1)
                ret_t(NST - 1)
                proj_tm_add(YT, (tYT,), "out", l, 8, X, tX)

                for _ in range(pc_per_tile):
                    if pcq:
                        pcq.pop(0)()
                norm_transpose(1, X, tX)
                for cc in range(2):
                    wa, twa = wbuf(); wload(wa, twa, "cq", l, cc * 512, 8, 512)
                    for c4 in range(4):
                        p, tp = ps()
                        for kt in range(8):
                            S.op("pe", lambda e, c4=c4, kt=kt, p=p, wa=wa: e.matmul(p[:, :], lhsT=wa[:, kt, c4 * 128:(c4 + 1) * 128], rhs=XT[:, kt, :], start=(kt == 0), stop=(kt == 7)), (twa, tXT), (tp,))
                        S.op("act", lambda e, cc=cc, c4=c4, p=p: e.activation(out=QC[:, cc * 4 + c4, :], in_=p[:, :], func=AF.Copy, scale=1.0 / 16.0), (tp,), tuple(tQC))
                MX = SMALL[:, 0:4]; SM = RSa[0]; tSM = tBN

                def xa_scores(st):
                    cs = slice(st * 128, (st + 1) * 128)
                    pa, tpa = ps(); pb, tpb = ps()
                    for h in range(4):
                        pp, tpp = (pa, tpa) if h < 2 else (pb, tpb)
                        o = pp[:, (h % 2) * 256:(h % 2 + 1) * 256]
                        for hf in range(2):
                            S.op("pe", lambda e, h=h, hf=hf, o=o: e.matmul(o, lhsT=QC[:, 2 * h + hf, cs], rhs=KM[:, 2 * h + hf, :], start=(hf == 0), stop=(hf == 1)), tuple(tQC) + (tKV,), (tpp,))
                    return ((pa, tpa), (pb, tpb))

                def xa_softmax(st, banks):
                    PNb, tPNb = PN2[st % 2], tPN2[st % 2]
                    for i, (pp, tpp) in enumerate(banks):
                        S.op("dve", lambda e, i=i, pp=pp: e.tensor_reduce(out=MX[:, i:i + 1], in_=pp[:, :], axis=AX.X, op=ALU.max), (tpp,), (tSMALL,))
                    S.op("dve", lambda e: e.tensor_tensor(out=MX[:, 2:3], in0=MX[:, 0:1], in1=MX[:, 1:2], op=ALU.max), (tSMALL,), (tSMALL,))
                    S.op("dve", lambda e: e.tensor_scalar(out=MX[:, 3:4], in0=MX[:, 2:3], scalar1=-1.0, scalar2=None, op0=ALU.mult), (tSMALL,), (tSMALL,))
                    for h in range(4):
                        pp, tpp = banks[h // 2]
                        S.op("act", lambda e, h=h, pp=pp: e.activation(out=EX[:, h * 256:(h + 1) * 256], in_=pp[:, (h % 2) * 256:(h % 2 + 1) * 256], func=AF.Exp, bias=MX[:, 3:4], accum_out=SM[:, h:h + 1]),
                             (tpp, tSMALL), tuple(tEX) + (tSM,))
                    S.op("dve", lambda e: e.reciprocal(out=SM, in_=SM), (tSM,), (tSM,))
                    S.op("pool", lambda e: e.tensor_tensor(out=PNb.rearrange("p (h m) -> p h m", h=4), in0=EX.rearrange("p (h m) -> p h m", h=4), in1=SM.unsqueeze(2).broadcast_to([128, 4, 256]), op=ALU.mult),
                         tuple(tEX) + (tSM,), (tPNb,))

                def xa_pv(st):
                    cs = slice(st * 128, (st + 1) * 128)
                    PNb, tPNb = PN2[st % 2], tPN2[st % 2]
                    pt, tpt = pst()
                    for i8 in range(8):
                        S.op("pe", lambda e, i8=i8, pt=pt: e.transpose(out=pt[:, i8 * 128:(i8 + 1) * 128], in_=PNb[:, i8 * 128:(i8 + 1) * 128], identity=IDB), (tPNb, tCST), (tpt,))
                    copy("act", PNT, pt.rearrange("p (a b) -> p a b", a=8), (tpt,), tuple(tPNT))
                    for half in range(2):
                        po, tpo = ps()
                        for h2 in range(2):
                            h = half * 2 + h2
                            for eh in range(2):
                                o = po[:, (h2 * 2 + eh) * 128:(h2 * 2 + eh + 1) * 128]
                                for mh in range(2):
                                    S.op("pe", lambda e, h=h, eh=eh, mh=mh, o=o: e.matmul(o, lhsT=VM[:, mh, h * 256 + eh * 128:h * 256 + (eh + 1) * 128], rhs=PNT[:, 2 * h + mh, :], start=(mh == 0), stop=(mh == 1)),
                                         (tKV,) + tuple(tPNT), (tpo,))
                        copy(ev_eng(), OT[:, half * 4:(half + 1) * 4, cs], po[:, :].rearrange("p (a b) -> p a b", a=4), (tpo,), tuple(tOT))
                bk = xa_scores(0)
                for st in range(NST):
                    nbk = xa_scores(st + 1) if st + 1 < NST else None
                    xa_softmax(st, bk)
                    xa_pv(st)
                    bk = nbk
                proj_tm_add(OT, tuple(tOT), "co", l, 8, X, tX)

                norm_transpose(2, X, tX)
                for fc in range(11):
                    wa, twa = wbuf()
                    S.dma("sp", lambda e, fc=fc, wa=wa: e.dma_start(out=wa[:, :, 0:256], in_=wbf["g"][l, fc]), wkey[id(twa)], (tW[("g", l)],), (twa,))
                    S.dma("sp", lambda e, fc=fc, wa=wa: e.dma_start(out=wa[:, :, 256:512], in_=wbf["u"][l, fc]), wkey[id(twa)], (tW[("u", l)],), (twa,))
                    for f2 in range(2):
                        f = fc * 2 + f2
                        pg, tpg = ps(); pu, tpu = ps()
                        for kt in range(8):
                            S.op("pe", lambda e, f2=f2, kt=kt, pg=pg, wa=wa: e.matmul(pg[:, :], lhsT=wa[:, kt, f2 * 128:(f2 + 1) * 128], rhs=XT[:, kt, :], start=(kt == 0), stop=(kt == 7)), (twa, tXT), (tpg,))
                        for kt in range(8):
                            S.op("pe", lambda e, f2=f2, kt=kt, pu=pu, wa=wa: e.matmul(pu[:, :], lhsT=wa[:, kt, 256 + f2 * 128:256 + (f2 + 1) * 128], rhs=XT[:, kt, :], start=(kt == 0), stop=(kt == 7)), (twa, tXT), (tpu,))
                        S.op("act", lambda e, pg=pg: e.activation(out=SC[5][:, 0:512], in_=pg[:, :], func=AF.Silu), (tpg,), (tSC[5],))
                        S.op("dve", lambda e, f=f, pu=pu: e.tensor_tensor(out=H[:, f, :], in0=SC[5][:, 0:512], in1=pu[:, :], op=ALU.mult), (tpu, tSC[5]), tuple(tH[:-1]))
                for c4 in range(4):
                    wd_, twd = WD[c4], tWDs[c4]
                    S.dma("sp", lambda e, c4=c4, wd_=wd_: e.dma_start(out=wd_, in_=wbf["d"][l, c4]), "wd%d" % c4, (tW[("d", l)],), twd)
                    for st in range(NST):
                        p, tp = ps()
                        for f in range(NF):
                            S.op("pe", lambda e, st=st, f=f, p=p, wd_=wd_: e.matmul(p[:, 0:256], lhsT=H[:, f, st * 128:(st + 1) * 128], rhs=wd_[:, f, :], start=(f == 0), stop=(f == NF - 1)),
                                 tuple(tH[:-1]) + twd, (tp,))
                        S.op("dve", lambda e, st=st, c4=c4, p=p: e.tensor_tensor(out=X[:, st, c4 * 256:(c4 + 1) * 256], in0=p[:, 0:256], in1=X[:, st, c4 * 256:(c4 + 1) * 256], op=ALU.add),
                             (tp, tX[st]), (tX[st],))

                def emit_out():
                    if l < NL - 1:
                        for st in range(NST):
                            S.dma("pool", lambda e, st=st: e.dma_start(out=xs_d[r0 + st * 128:r0 + (st + 1) * 128, :], in_=X[:, st, :]), "xo%d" % st, (tX[st],), ())
                    else:
                        for st in range(NST):
                            S.dma("pool", lambda e, st=st: e.dma_start(out=out_d[r0 + st * 128:r0 + (st + 1) * 128, :], in_=X[:, st, :]), "xo%d" % st, (tX[st],), ())
                if l == NL - 1:
                    if ti == 0:
                        S.dma("pool", lambda e: e.dma_start(out=GFIN, in_=nfin_d[0:1, :].partition_broadcast(128)), "gfin", (), (tGFIN,))
                    for st in range(NST):
                        S.op("act", lambda e, st=st: e.activation(out=JUNK, in_=X[:, st, :], func=AF.Square, accum_out=SS[:, st:st + 1]), (tX[st],), (tSC[5], tSS))
                    S.op("dve", lambda e: e.tensor_scalar(out=SS[:, 4:8], in0=SS[:, 0:4], scalar1=1.0 / D, scalar2=EPS, op0=ALU.mult, op1=ALU.add), (tSS,), (tSS,))
                    S.op("act", lambda e: e.activation(out=SS[:, 4:8], in_=SS[:, 4:8], func=AF.Sqrt), (tSS,), (tSS,))
                    S.op("dve", lambda e: e.reciprocal(out=SS[:, 4:8], in_=SS[:, 4:8]), (tSS,), (tSS,))
                    for st in range(NST):
                        S.op("dve", lambda e, st=st: e.scalar_tensor_tensor(out=X[:, st, :], in0=X[:, st, :], scalar=SS[:, 4 + st:5 + st], in1=GFIN, op0=ALU.mult, op1=ALU.mult),
                             (tX[st], tSS, tGFIN), (tX[st],))
                pending.append(emit_out)

            for ti in range(NT):
                do_tile(ti)
            while pending:
                pending.pop(0)()
            while pcq:
                pcq.pop(0)()

        for l in range(NL):
            do_layer(l)
        S.barrier()
        print("instr counts:", {e: len(S.prog[e]) for e in ENG})
        block = stack.enter_context(nc.Block())
        S.emit(block)
    return nc


def make_inmap(inputs, b, NT, NL):
    T = NT * TT
    f = lambda a: np.ascontiguousarray(np.asarray(a, dtype=np.float32))
    w_in = np.asarray(inputs["w_in"], dtype=np.float32)[:NL]
    swap = np.concatenate([np.arange(h * 128 + 64, h * 128 + 128).tolist() + np.arange(h * 128, h * 128 + 64).tolist() for h in range(4)]).astype(np.int64)
    qs = w_in[:, :, 512:1024][:, :, swap]
    ks = w_in[:, :, 1024:1536][:, :, swap]
    w_in_ext = np.ascontiguousarray(np.concatenate([w_in, qs, ks], axis=2))
    m = {
        "x": f(inputs["x"][b, :T]),
        "pos": np.ascontiguousarray(np.asarray(inputs["positions"])[b:b + 1, :T].astype(np.int32)),
        "mem": f(inputs["mem"][b]),
        "norm_mix": f(inputs["norm_mix"][:NL]), "norm_cross": f(inputs["norm_cross"][:NL]),
        "norm_mem": f(inputs["norm_mem"][:NL]), "norm_ffn": f(inputs["norm_ffn"][:NL]),
        "norm_final": f(np.asarray(inputs["norm_final"]).reshape(1, D)),
        "w_in": w_in_ext,
        "lam_re": f(inputs["s5_lambda_re"][:NL]), "lam_im": f(inputs["s5_lambda_im"][:NL]), "log_step": f(inputs["s5_log_step"][:NL]),
        "b_re": f(inputs["s5_b_re"][:NL]), "b_im": f(inputs["s5_b_im"][:NL]),
        "c_re": f(inputs["s5_c_re"][:NL]), "c_im": f(inputs["s5_c_im"][:NL]),
        "s5_d": f(inputs["s5_d"][:NL]), "w_glu": f(inputs["s5_w_glu"][:NL]), "b_glu": f(inputs["s5_b_glu"][:NL]),
        "s5_out_norm": f(inputs["s5_out_norm"][:NL]), "ret_out_norm": f(np.asarray(inputs["ret_out_norm"])[:NL].reshape(NL, 512)),
        "w_out": f(inputs["w_out"][:NL]), "w_cq": f(inputs["w_cq"][:NL]), "w_ck": f(inputs["w_ck"][:NL]),
        "w_cv": f(inputs["w_cv"][:NL]), "w_co": f(inputs["w_co"][:NL]),
        "w_gate": f(inputs["w_gate"][:NL]), "w_up": f(inputs["w_up"][:NL]), "w_down": f(inputs["w_down"][:NL]),
        "cst": host_consts(),
    }
    return m


def kernel(**inputs):
    NT, NL = 16, 4
    nc = build_program(NT, NL)
    base = make_inmap(inputs, 0, NT, NL)
    in_maps = []
    for b in range(4):
        m = dict(base)
        m["x"] = np.ascontiguousarray(np.asarray(inputs["x"][b], dtype=np.float32))
        m["pos"] = np.ascontiguousarray(np.asarray(inputs["positions"])[b:b + 1].astype(np.int32))
        m["mem"] = np.ascontiguousarray(np.asarray(inputs["mem"][b], dtype=np.float32))
        in_maps.append(m)
    res = run_bass_kernel_spmd(nc, in_maps, core_ids=list(range(4)))
    out = np.stack([np.asarray(r["out"], dtype=np.float32) for r in res.results], axis=0)
    return out
```

```python
import math
from contextlib import ExitStack
import numpy as np
import concourse.bass as bass
import concourse.mybir as mybir
from concourse.bass_utils import run_bass_kernel_spmd
from concourse.alu_op_type import AluOpType as ALU

F32 = mybir.dt.float32
BF16 = mybir.dt.bfloat16
I32 = mybir.dt.int32
U8 = mybir.dt.uint8
AF = mybir.ActivationFunctionType
AX = mybir.AxisListType

D = 1024
TT = 512
NST = 4
NB = 64
DFF = 2816
NF = 22
EPS = 1e-6
SAME_RAW = True
ENG = ["pe", "act", "dve", "pool", "sp"]
TWO_PI = 2.0 * math.pi
CW1 = 6.28125
CW2 = TWO_PI - 6.28125
PI_SAFE = 3.1415925


class Tok:
    __slots__ = ("name", "w", "r")

    def __init__(self, name):
        self.name = name
        self.w = {}
        self.r = {}


class Sched:
    def __init__(self, nc, stack):
        self.nc = nc
        self.stack = stack
        self.prog = {e: [] for e in ENG}
        self.cnt = {e: 0 for e in ENG}
        self.seen = {e: {} for e in ENG}
        self.sems = {}
        self.dcnt = {}
        for e in ENG:
            self.sems["e:" + e] = stack.enter_context(nc.semaphore("s_" + e))

    def _need(self, eng, key, val, same_ok):
        if key == "e:" + eng and not same_ok:
            return
        if self.seen[eng].get(key, 0) >= val:
            return
        self.seen[eng][key] = val
        self.prog[eng].append(("wait", key, val))

    def _deps(self, eng, reads, writes):
        for t in reads:
            for k, v in t.w.items():
                self._need(eng, k, v, SAME_RAW)
        for t in writes:
            for k, v in t.w.items():
                self._need(eng, k, v, False)
            for k, v in t.r.items():
                self._need(eng, k, v, False)

    def op(self, eng, fn, reads=(), writes=()):
        self._deps(eng, reads, writes)
        self.cnt[eng] += 1
        c = self.cnt[eng]
        key = "e:" + eng
        self.prog[eng].append(("op", fn, key, 1))
        for t in reads:
            t.r[key] = c
        for t in writes:
            t.w[key] = c

    def dma(self, q, fn, dkey, reads=(), writes=()):
        self._deps(q, reads, writes)
        key = "d:" + dkey
        if key not in self.sems:
            self.sems[key] = self.stack.enter_context(self.nc.semaphore("d_" + dkey))
            self.dcnt[key] = 0
        self.dcnt[key] += 16
        v = self.dcnt[key]
        self.prog[q].append(("op", fn, key, 16))
        for t in reads:
            t.r[key] = v
        for t in writes:
            t.w[key] = v

    def barrier(self):
        for e in ENG:
            for f in ENG:
                if f != e and self.cnt[f] > 0:
                    self._need(e, "e:" + f, self.cnt[f], False)
            for k, v in self.dcnt.items():
                if not k.startswith("d:pc_"):
                    self._need(e, k, v, False)

    def emit(self, block):
        def run(eng_name):
            def body(e):
                for item in self.prog[eng_name]:
                    if item[0] == "wait":
                        e.wait_ge(self.sems[item[1]], item[2])
                    else:
                        inst = item[1](e)
                        inst.then_inc(self.sems[item[2]], item[3])
            return body
        block.tensor(run("pe"))
        block.scalar(run("act"))
        block.vector(run("dve"))
        block.gpsimd(run("pool"))
        block.sync(run("sp"))


def host_consts():
    c = np.zeros((128, 1114), np.float32)
    c[:, 0:128] = np.eye(128, dtype=np.float32)
    s_idx = np.arange(128) // 16
    c[:, 128:256] = (s_idx[None, :] >= s_idx[:, None]).astype(np.float32)
    idx = np.arange(128, dtype=np.float64)
    for h in range(4):
        lg = math.log1p(-2.0 ** (-5.0 - h))
        m = np.where(idx[:, None] <= idx[None, :], np.exp(-(idx[:, None] + 1.0) * lg), 0.0) * (128.0 ** -0.5)
        c[:, 256 + h * 128:256 + (h + 1) * 128] = m.astype(np.float32)
        c[:, 768 + h] = np.exp((idx + 1.0) * lg)
        c[:, 772 + h] = np.exp((127.0 - idx) * lg) * (128.0 ** -0.5)
    i = np.arange(128) % 64
    c[:, 776] = 1.0 / (10000.0 ** (i.astype(np.float64) / 64.0))
    c[:, 777] = np.where(np.arange(128) < 64, -1.0, 1.0)
    c[:, 778:842] = 8.0 * np.arange(1, 65, dtype=np.float32)[None, :]
    c[:, 842:858] = np.arange(-7, 9, dtype=np.float32)[None, :]
    c[:, 858:986] = 1.0
    for h in range(16):
        c[h, 986 + np.arange(8) * 16 + h] = 1.0
    return c


GAMMA128 = [math.exp(128.0 * math.log1p(-2.0 ** (-5.0 - h))) for h in range(4)]


def build_program(NT, NL, dbg=False):
    nc = bass.Bass("TRN2", target_bir_lowering=False)
    T = NT * TT
    dr = {}

    def din(name, shape, dt=F32):
        dr[name] = nc.dram_tensor(name, list(shape), dt, kind="ExternalInput").ap()
        return dr[name]

    x_d = din("x", [T, D])
    pos_d = din("pos", [1, T], I32)
    mem_d = din("mem", [256, D])
    nmix_d = din("norm_mix", [NL, D]); ncross_d = din("norm_cross", [NL, D])
    nmem_d = din("norm_mem", [NL, D]); nffn_d = din("norm_ffn", [NL, D]); nfin_d = din("norm_final", [1, D])
    win_d = din("w_in", [NL, D, 3584])
    lre_d = din("lam_re", [NL, 32, 64]); lim_d = din("lam_im", [NL, 32, 64]); lst_d = din("log_step", [NL, 32])
    bre_d = din("b_re", [NL, 32, 64, 16]); bim_d = din("b_im", [NL, 32, 64, 16])
    cre_d = din("c_re", [NL, 32, 16, 64]); cim_d = din("c_im", [NL, 32, 16, 64])
    sd_d = din("s5_d", [NL, 512]); wglu_d = din("w_glu", [NL, 512, 512]); bglu_d = din("b_glu", [NL, 512])
    s5n_d = din("s5_out_norm", [NL, 512]); rn_d = din("ret_out_norm", [NL, 512])
    wout_d = din("w_out", [NL, D, D]); wcq_d = din("w_cq", [NL, D, D]); wck_d = din("w_ck", [NL, D, D])
    wcv_d = din("w_cv", [NL, D, D]); wco_d = din("w_co", [NL, D, D])
    wg_d = din("w_gate", [NL, D, DFF]); wu_d = din("w_up", [NL, D, DFF]); wd_d = din("w_down", [NL, DFF, D])
    cst_d = din("cst", [128, 1114])
    out_d = nc.dram_tensor("out", [T, D], F32, kind="ExternalOutput").ap()
    xs_d = nc.dram_tensor("xscr", [T, D], F32, kind="Internal").ap()
    rot_d = nc.dram_tensor("rotscr", [NT, 2, 128, TT], F32, kind="Internal").ap()
    wsrc = {"in": win_d, "out": wout_d, "cq": wcq_d, "ck": wck_d, "cv": wcv_d, "co": wco_d, "g": wg_d, "u": wu_d, "d": wd_d, "glu": wglu_d}
    wchk = {"in": (7, 8, 512), "out": (2, 8, 512), "cq": (2, 8, 512), "ck": (2, 8, 512), "cv": (2, 8, 512), "co": (2, 8, 512),
            "g": (11, 8, 256), "u": (11, 8, 256), "d": (4, NF, 256), "glu": (1, 4, 512)}
    wbf = {k: nc.dram_tensor("wbf_" + k, [NL, wchk[k][0], 128, wchk[k][1], wchk[k][2]], BF16, kind="Internal").ap() for k in wsrc}
    tW = {(k, l): Tok("w_%s_%d" % (k, l)) for k in wsrc for l in range(NL)}

    stack = ExitStack()
    with stack:
        arena = stack.enter_context(nc.sbuf_tensor("arena", [128, 206 * 1024], U8))
        S = Sched(nc, stack)
        off = [0]

        def alloc(shape, dt, at=None):
            esz = 4 if dt in (F32, I32) else 2
            n = int(np.prod(shape[1:]))
            nbytes = n * esz
            o = off[0] if at is None else at
            if at is None:
                off[0] = (off[0] + nbytes + 63) // 64 * 64
            assert o + nbytes <= 206 * 1024, (o, nbytes)
            v = arena[0:shape[0], o:o + nbytes].bitcast(dt)
            if len(shape) == 3:
                v = v.rearrange("p (a b) -> p a b", a=shape[1])
            elif len(shape) == 4:
                v = v.rearrange("p (a b c) -> p a b c", a=shape[1], b=shape[2])
            return v

        CST = alloc([128, 1114], F32); tCST = Tok("cst")
        IDB = alloc([128, 128], BF16); ONESB = alloc([128, 128], BF16)
        GT3 = [alloc([128, D], F32) for _ in range(3)]; tGT3 = [Tok("gt%d" % i) for i in range(3)]
        GRET = alloc([128, 512], F32); GS5 = alloc([128, 4], F32); BGLU = alloc([128, 4], F32); tGS = Tok("gs")
        WGLU = alloc([128, 4, 512], BF16); tWGLU = Tok("wglu")
        G5 = alloc([128, 32, 128], BF16); WPRE = alloc([128, 16, 128], BF16); WPIM = alloc([128, 16, 128], BF16)
        VRE = alloc([128, 16, 128], BF16); VIM = alloc([128, 16, 128], BF16)
        COSR = alloc([128, 16, 64], F32); SINR = alloc([128, 16, 64], F32); RHO8 = alloc([128, 16], F32)
        tS5C = Tok("s5c")
        STRE = alloc([128, 16], F32); STIM = alloc([128, 16], F32); tST = Tok("s5state")
        RST = alloc([128, 4, 128], F32); RBF = alloc([128, 4, 128], BF16); tR = Tok("rstate")
        KM = alloc([128, 8, 256], BF16); VM = alloc([128, 2, D], BF16); tKV = Tok("memkv")
        SS = alloc([128, 8], F32); tSS = Tok("ss")
        SMALL = alloc([128, 64], F32); tSMALL = Tok("small")
        MVa = [alloc([128, 8], F32) for _ in range(2)]; RSa = [alloc([128, 4], F32) for _ in range(2)]
        wb_off = off[0]
        WB = [alloc([128, 8, 512], BF16) for _ in range(3)]; tWB = [Tok("wb%d" % i) for i in range(3)]
        G128T = alloc([128, 4, 128], F32)
        GFIN = alloc([128, D], F32); tGFIN = Tok("gfin")
        tr0 = off[0]
        XS = [alloc([128, NST, D], F32) for _ in range(2)]; tXS = [[Tok("x%d_%d" % (b, i)) for i in range(NST)] for b in range(2)]
        XT = alloc([128, 8, TT], BF16); tXT = Tok("xt")
        COS2 = alloc([128, TT], F32); SIN2 = alloc([128, TT], F32); tROT = Tok("rot")
        U8B = alloc([128, 32, 64], BF16); tU8 = Tok("u8")
        ZT = alloc([128, 4, TT], BF16); tZT = Tok("zt")
        SPRE = alloc([128, 16, 64], BF16); SPIM = alloc([128, 16, 64], BF16); tSP = Tok("sp")
        qt_off = off[0]
        QT = alloc([128, 4, TT], BF16); KT = alloc([128, 4, TT], BF16); tQT = Tok("qt"); tKT = Tok("kt")
        VV = alloc([128, NST, 512], BF16); tVV = Tok("vv")
        GG = alloc([128, NST, 512], BF16); tGG = Tok("gg")
        YT = alloc([128, 8, TT], BF16); tYT = Tok("yt")
        WD = [alloc([128, NF, 256], BF16, at=qt_off + i * 11264) for i in range(2)] + [alloc([128, NF, 256], BF16, at=wb_off + i * 11264) for i in range(2)]
        tWDs = [(tQT, tKT, tVV), (tVV, tGG, tYT), (tWB[0], tWB[1]), (tWB[1], tWB[2])]
        assert wb_off + 2 * 11264 <= wb_off + 3 * 8192
        assert qt_off + 2 * 11264 <= off[0]
        scr0 = off[0]
        TMUZ = alloc([128, 8, 512], BF16); tTM = Tok("tmuz")
        SC = [alloc([128, 1024], F32) for _ in range(4)] + [alloc([128, 512], F32) for _ in range(2)]
        tSC = [Tok("sc%d" % i) for i in range(6)]
        scr1 = off[0]
        tSC3b = Tok("sc3b"); tSC4b = Tok("sc4b")
        TMUv = TMUZ.rearrange("p t c -> p (t c)").rearrange("p (g t h) -> p g t h", g=32, t=8)
        XN = alloc([128, NST, D], BF16, at=scr0); tXN = [tTM] * NST
        JUNK = SC[5].bitcast(BF16)
        H = alloc([128, NF, TT], BF16, at=scr0); tH = [tTM] + tSC
        QC = alloc([128, 8, TT], BF16, at=scr0)
        OT = alloc([128, 8, TT], BF16, at=scr0 + 8192)
        EX = alloc([128, 1024], F32, at=scr0 + 16384)
        PN = alloc([128, 1024], BF16, at=scr0 + 20480)
        PNT = alloc([128, 8, 128], BF16, at=scr0 + 24576)
        tQC = [tTM]; tOT = [tSC[0], tSC[1]]; tEX = [tSC[2]]; tPN = [tSC[3]]; tPNT = [tSC[4], tSC4b]
        PN2 = [alloc([128, 1024], BF16, at=scr0 + 20480 + i * 2048) for i in range(2)]; tPN2 = [tSC[3], tSC3b]
        print("SBUF used bytes/partition:", off[0])

        PS = [stack.enter_context(nc.psum_tensor("ps%d" % i, [128, 512], F32)) for i in range(6)]
        tPS = [Tok("ps%d" % i) for i in range(6)]
        PTB = [stack.enter_context(nc.psum_tensor("pt%d" % i, [128, 1024], BF16)) for i in range(2)]
        tPT = [Tok("pt%d" % i) for i in range(2)]
        rr = [0, 0]

        def ps():
            i = rr[0] % 6; rr[0] += 1
            return PS[i], tPS[i]

        def pst():
            i = rr[1] % 2; rr[1] += 1
            return PTB[i], tPT[i]

        wrr = [0]

        def wbuf():
            i = wrr[0] % 3; wrr[0] += 1
            wkey[id(tWB[i])] = "wb%d" % i
            return WB[i], tWB[i]

        wkey = {}

        def precast_list(l):
            lst = []
            for k in ("ck", "cv", "glu", "in", "out", "cq", "co", "g", "u", "d"):
                nch, nkt, ncol = wchk[k]
                for c in range(nch):
                    def f(k=k, c=c, ncol=ncol):
                        S.dma("pool", lambda e: e.dma_start(out=wbf[k][l, c], in_=wsrc[k][l][:, c * ncol:(c + 1) * ncol].rearrange("(kt p) n -> p kt n", p=128)),
                              "pc_" + k, (), (tW[(k, l)],))
                    lst.append(f)
            return lst

        def precast(l):
            for f in precast_list(l):
                f()

        evr = [0]

        def ev_eng():
            evr[0] += 1
            return "act" if evr[0] % 2 == 0 else "dve"

        def copy(eng, out, in_, reads, writes):
            if eng == "act":
                S.op("act", lambda e: e.activation(out=out, in_=in_, func=AF.Copy), reads, writes)
            else:
                S.op("dve", lambda e: e.tensor_copy(out=out, in_=in_), reads, writes)

        def wload(buf, tok, name, l, c0, nkt, ncols, key=None):
            assert ncols == wchk[name][2] and c0 % ncols == 0
            src_ap = wbf[name][l, c0 // ncols]
            S.dma("sp", lambda e: e.dma_start(out=buf[:, 0:nkt, 0:ncols], in_=src_ap),
                  wkey[id(tok)], (tW[(name, l)],), (tok,))

        S.dma("pool", lambda e: e.dma_start(out=CST, in_=cst_d[:, :]), "cst", (), (tCST,))
        S.op("dve", lambda e: e.tensor_copy(out=IDB, in_=CST[:, 0:128]), (tCST,), (tCST,))
        S.op("dve", lambda e: e.tensor_copy(out=ONESB, in_=CST[:, 858:986]), (tCST,), (tCST,))
        IDF = CST[:, 0:128]
        for h in range(4):
            S.op("dve", lambda e, h=h: e.memset(G128T[:, h, :], GAMMA128[h]), (), (tCST,))

        def range_reduce(ang, tmpi, tmpf, n, toks):
            S.op("dve", lambda e: e.tensor_scalar(out=tmpi, in0=ang, scalar1=1.0 / TWO_PI, scalar2=None, op0=ALU.mult), toks, toks)
            S.op("dve", lambda e: e.tensor_copy(out=tmpf, in_=tmpi), toks, toks)
            S.op("dve", lambda e: e.scalar_tensor_tensor(out=ang, in0=tmpf, scalar=-CW1, in1=ang, op0=ALU.mult, op1=ALU.add), toks, toks)
            S.op("dve", lambda e: e.scalar_tensor_tensor(out=ang, in0=tmpf, scalar=-CW2, in1=ang, op0=ALU.mult, op1=ALU.add), toks, toks)
            S.op("dve", lambda e: e.tensor_scalar(out=ang, in0=ang, scalar1=PI_SAFE, scalar2=-PI_SAFE, op0=ALU.min, op1=ALU.max), toks, toks)

        def load_gain(row_ap, gi=0):
            S.dma("pool", lambda e: e.dma_start(out=GT3[gi], in_=row_ap.partition_broadcast(128)), "gt%d" % gi, (), (tGT3[gi],))

        def norm_transpose(gi, X, tX):
            gtab, tg = GT3[gi], tGT3[gi]
            for st in range(NST):
                S.op("act", lambda e, st=st: e.activation(out=JUNK, in_=X[:, st, :], func=AF.Square, accum_out=SS[:, st:st + 1]),
                     (tX[st],), (tSC[5], tSS))
            S.op("dve", lambda e: e.tensor_scalar(out=SS[:, 4:8], in0=SS[:, 0:4], scalar1=1.0 / D, scalar2=EPS, op0=ALU.mult, op1=ALU.add), (tSS,), (tSS,))
            S.op("act", lambda e: e.activation(out=SS[:, 4:8], in_=SS[:, 4:8], func=AF.Sqrt), (tSS,), (tSS,))
            S.op("dve", lambda e: e.reciprocal(out=SS[:, 4:8], in_=SS[:, 4:8]), (tSS,), (tSS,))
            for st in range(NST):
                S.op("dve", lambda e, st=st: e.scalar_tensor_tensor(out=XN[:, st, :], in0=X[:, st, :], scalar=SS[:, 4 + st:5 + st], in1=gtab,
                                                                      op0=ALU.mult, op1=ALU.mult), (tX[st], tSS, tg), (tXN[st],))
            for st in range(NST):
                pt, tpt = pst()
                for kt in range(8):
                    S.op("pe", lambda e, st=st, kt=kt, pt=pt: e.transpose(out=pt[:, kt * 128:(kt + 1) * 128], in_=XN[:, st, kt * 128:(kt + 1) * 128], identity=IDB),
                         (tXN[st], tCST), (tpt,))
                copy(ev_eng(), XT[:, :, st * 128:(st + 1) * 128], pt.rearrange("p (k t) -> p k t", k=8), (tpt,), (tXT,))

        def proj_tm_add(lhs, tl, wname, l, nk, X, tX):
            for cc in range(2):
                wb, twb = wbuf()
                wload(wb, twb, wname, l, cc * 512, nk, 512)
                for st in range(NST):
                    p, tp = ps()
                    for kt in range(nk):
                        S.op("pe", lambda e, st=st, kt=kt, p=p, wb=wb: e.matmul(p[:, :], lhsT=lhs[:, kt, st * 128:(st + 1) * 128], rhs=wb[:, kt, :],
                                                                                  start=(kt == 0), stop=(kt == nk - 1)), tl + (twb,), (tp,))
                    S.op("dve", lambda e, st=st, cc=cc, p=p: e.tensor_tensor(out=X[:, st, cc * 512:(cc + 1) * 512], in0=p[:, :], in1=X[:, st, cc * 512:(cc + 1) * 512], op=ALU.add),
                         (tp, tX[st]), (tX[st],))

        def rot_build(ti):
            r0 = ti * TT
            PI_ = SC[0].bitcast(I32)[:, 0:TT]
            S.dma("sp", lambda e: e.dma_start(out=PI_, in_=pos_d[0:1, r0:r0 + TT].partition_broadcast(128)), "pos", (), (tSC[0],))
            S.op("dve", lambda e: e.tensor_copy(out=SC[1][:, 0:TT], in_=PI_), (tSC[0],), (tSC[1],))
            S.op("dve", lambda e: e.tensor_scalar(out=SIN2, in0=SC[1][:, 0:TT], scalar1=CST[:, 776:777], scalar2=None, op0=ALU.mult), (tSC[1], tCST), (tROT,))
            S.op("dve", lambda e: e.tensor_scalar(out=COS2, in0=SIN2, scalar1=math.pi / 2, scalar2=None, op0=ALU.add), (tROT,), (tROT,))
            for tab in (SIN2, COS2):
                range_reduce(tab, SC[0].bitcast(I32)[:, 0:TT], SC[1][:, 0:TT], 0, (tROT, tSC[0], tSC[1]))
            S.op("act", lambda e: e.activation(out=SIN2, in_=SIN2, func=AF.Sin, scale=CST[:, 777:778]), (tROT, tCST), (tROT,))
            S.op("act", lambda e: e.activation(out=COS2, in_=COS2, func=AF.Sin), (tROT,), (tROT,))


            S.dma("sp", lambda e: e.dma_start(out=rot_d[ti, 0], in_=SIN2), "rot", (tROT,), ())
            S.dma("sp", lambda e: e.dma_start(out=rot_d[ti, 1], in_=COS2), "rot", (tROT,), ())

        precast(0)
        for ti_ in range(NT):
            rot_build(ti_)

        def do_layer(l):
            S.barrier()
            pcq = precast_list(l + 1) if l + 1 < NL else []
            pc_per_tile = (len(pcq) + NT - 1) // NT
            S.dma("pool", lambda e: e.dma_start(out=GRET, in_=rn_d[l:l + 1, :].partition_broadcast(128)), "gs", (), (tGS,))
            S.dma("pool", lambda e: e.dma_start(out=GS5, in_=s5n_d[l, :].rearrange("(c p) -> p c", p=128), allow_slow_non_contiguous=True), "gs", (), (tGS,))
            S.dma("pool", lambda e: e.dma_start(out=BGLU, in_=bglu_d[l, :].rearrange("(c p) -> p c", p=128), allow_slow_non_contiguous=True), "gs", (), (tGS,))
            S.dma("sp", lambda e: e.dma_start(out=WGLU, in_=wbf["glu"][l, 0]), "wglu", (tW[("glu", l)],), (tWGLU,))
            S.op("dve", lambda e: e.memset(STRE, 0.0), (), (tST,))
            S.op("dve", lambda e: e.memset(STIM, 0.0), (), (tST,))
            S.op("dve", lambda e: e.memset(RST, 0.0), (), (tR,))
            S.op("dve", lambda e: e.memset(RBF, 0.0), (), (tR,))

            tS = (tSC[0], tSC[1], tSC[2], tSC[3], tSC[4], tSC[5], tTM)
            MEMX = alloc([128, 2, D], F32, at=scr0 + 8192)
            MEMN = alloc([128, 2, D], BF16, at=scr0 + 16384)
            MT = alloc([128, 8, 256], BF16, at=scr0 + 20480)
            GMEM = GT3[0]
            S.dma("pool", lambda e: e.dma_start(out=MEMX, in_=mem_d.rearrange("(a p) d -> p a d", p=128)), "setup", (), tS)
            load_gain(nmem_d[l:l + 1, :], 0)
            for a in range(2):
                S.op("act", lambda e, a=a: e.activation(out=JUNK, in_=MEMX[:, a, :], func=AF.Square, accum_out=SS[:, a:a + 1]), tS, tS + (tSS,))
            S.op("dve", lambda e: e.tensor_scalar(out=SS[:, 4:6], in0=SS[:, 0:2], scalar1=1.0 / D, scalar2=EPS, op0=ALU.mult, op1=ALU.add), (tSS,), (tSS,))
            S.op("act", lambda e: e.activation(out=SS[:, 4:6], in_=SS[:, 4:6], func=AF.Sqrt), (tSS,), (tSS,))
            S.op("dve", lambda e: e.reciprocal(out=SS[:, 4:6], in_=SS[:, 4:6]), (tSS,), (tSS,))
            for a in range(2):
                S.op("dve", lambda e, a=a: e.scalar_tensor_tensor(out=MEMN[:, a, :], in0=MEMX[:, a, :], scalar=SS[:, 4 + a:5 + a], in1=GMEM, op0=ALU.mult, op1=ALU.mult),
                     tS + (tSS, tGT3[0]), tS)
            for a in range(2):
                pt, tpt = pst()
                for kt in range(8):
                    S.op("pe", lambda e, a=a, kt=kt, pt=pt: e.transpose(out=pt[:, kt * 128:(kt + 1) * 128], in_=MEMN[:, a, kt * 128:(kt + 1) * 128], identity=IDB), tS + (tCST,), (tpt,))
                copy("dve", MT[:, :, a * 128:(a + 1) * 128], pt.rearrange("p (k t) -> p k t", k=8), (tpt,), tS)
            for cc in range(2):
                wb, twb = wbuf()
                wload(wb, twb, "ck", l, cc * 512, 8, 512)
                for c4 in range(4):
                    p, tp = ps()
                    for kt in range(8):
                        S.op("pe", lambda e, kt=kt, c4=c4, p=p, wb=wb: e.matmul(p[:, 0:256], lhsT=wb[:, kt, c4 * 128:(c4 + 1) * 128], rhs=MT[:, kt, :], start=(kt == 0), stop=(kt == 7)),
                             tS + (twb,), (tp,))
                    copy(ev_eng(), KM[:, cc * 4 + c4, :], p[:, 0:256], (tp,), (tKV,))
            for cc in range(2):
                wb, twb = wbuf()
                wload(wb, twb, "cv", l, cc * 512, 8, 512)
                for a in range(2):
                    p, tp = ps()
                    for kt in range(8):
                        S.op("pe", lambda e, kt=kt, a=a, p=p, wb=wb: e.matmul(p[:, :], lhsT=MT[:, kt, a * 128:(a + 1) * 128], rhs=wb[:, kt, :], start=(kt == 0), stop=(kt == 7)),
                             tS + (twb,), (tp,))
                    copy(ev_eng(), VM[:, a, cc * 512:(cc + 1) * 512], p[:, :], (tp,), (tKV,))

            so = [tr0]

            def sal(shape, dt):
                esz = 4 if dt in (F32, I32) else 2
                nb = int(np.prod(shape[1:])) * esz
                v = alloc(shape, dt, at=so[0])
                so[0] = (so[0] + nb + 63) // 64 * 64
                assert so[0] <= scr0, (so[0], scr0)
                return v
            tQ = (tXT, tROT, tU8, tZT, tSP, tQT, tKT, tVV, tGG, tYT) + tuple(tXS[0]) + tuple(tXS[1])
            LR = sal([128, 16], F32); LI = sal([128, 16], F32); STP = sal([128, 16], F32)
            LRS = sal([128, 16], F32); TH = sal([128, 16], F32); DEN = sal([128, 16], F32)
            FR = sal([128, 16], F32); FI = sal([128, 16], F32); A1R = sal([128, 16], F32); A1I = sal([128, 16], F32)
            T0 = sal([128, 16], F32); T1 = sal([128, 16], F32)
            APR = sal([128, 16, 16], F32); API = sal([128, 16, 16], F32)
            ANG = sal([128, 16, 16], F32); ANGI = sal([128, 16, 16], I32); ANGF = sal([128, 16, 16], F32); MAGK = sal([128, 16, 16], F32)
            LSB = sal([128, 32], F32)
            BR = sal([128, 16, 16], F32); BI = sal([128, 16, 16], F32); BBR = sal([128, 16, 16], F32); BBI = sal([128, 16, 16], F32)
            TB0 = sal([128, 16, 16], F32); TB1 = sal([128, 16, 16], F32)
            CNR = sal([16, 16, 128], F32); CNI = sal([16, 16, 128], F32)
            CTR = sal([128, 16, 16], F32); CTI = sal([128, 16, 16], F32)
            DM = sal([16, 32], F32); D8 = sal([128, 32], F32)
            W1 = sal([128, 16, 128], F32); W2 = sal([128, 16, 128], F32)
            WTR = sal([128, 16, 128], BF16); WTI = sal([128, 16, 128], BF16)
            XRB = sal([128, 16, 128], BF16); XIB = sal([128, 16, 128], BF16)
            ANGR = sal([128, 16, 64], F32); ANGRI = sal([128, 16, 64], I32); ANGRF = sal([128, 16, 64], F32)

            def ld_qp(dst, src2d):
                S.dma("pool", lambda e: e.dma_start(out=dst, in_=src2d.rearrange("(P r) p -> (r p) P", r=2), allow_slow_non_contiguous=True), "setup", (), tQ)
            ld_qp(LR, lre_d[l]); ld_qp(LI, lim_d[l])
            for r in range(2):
                S.dma("pool", lambda e, r=r: e.dma_start(out=LSB[r * 64:(r + 1) * 64, :], in_=lst_d[l:l + 1, :].partition_broadcast(64)), "setup", (), tQ)
                S.dma("pool", lambda e, r=r: e.dma_start(out=BR[r * 64:(r + 1) * 64], in_=bre_d[l].rearrange("(P r) p h -> r p P h", r=2)[r]), "setup", (), tQ)
                S.dma("pool", lambda e, r=r: e.dma_start(out=BI[r * 64:(r + 1) * 64], in_=bim_d[l].rearrange("(P r) p h -> r p P h", r=2)[r]), "setup", (), tQ)
            S.dma("pool", lambda e: e.dma_start(out=CNR, in_=cre_d[l].rearrange("(P r) n p -> n P r p", r=2)), "setup", (), tQ)
            S.dma("pool", lambda e: e.dma_start(out=CNI, in_=cim_d[l].rearrange("(P r) n p -> n P r p", r=2)), "setup", (), tQ)
            S.dma("pool", lambda e: e.dma_start(out=DM, in_=sd_d[l, :].rearrange("(g h) -> h g", h=16), allow_slow_non_contiguous=True), "setup", (), tQ)

            def dv(fn):
                S.op("dve", fn, tQ + (tCST,), tQ)

            def ac(fn):
                S.op("act", fn, tQ + (tCST,), tQ)
            for r in range(2):
                ac(lambda e, r=r: e.activation(out=STP[r * 64:(r + 1) * 64, :], in_=LSB[r * 64:(r + 1) * 64, r::2], func=AF.Exp))
            dv(lambda e: e.tensor_tensor(out=LRS, in0=LR, in1=STP, op=ALU.mult))
            dv(lambda e: e.tensor_tensor(out=TH, in0=LI, in1=STP, op=ALU.mult))
            KP = CST[:, 842:858]
            dv(lambda e: e.tensor_tensor(out=ANG, in0=TH.unsqueeze(2).broadcast_to([128, 16, 16]), in1=KP.unsqueeze(1).broadcast_to([128, 16, 16]), op=ALU.mult))
            dv(lambda e: e.tensor_tensor(out=MAGK, in0=LRS.unsqueeze(2).broadcast_to([128, 16, 16]), in1=KP.unsqueeze(1).broadcast_to([128, 16, 16]), op=ALU.mult))
            ac(lambda e: e.activation(out=MAGK, in_=MAGK, func=AF.Exp))
            dv(lambda e: e.tensor_scalar(out=ANGF, in0=ANG, scalar1=math.pi / 2, scalar2=None, op0=ALU.add))
            dv(lambda e: e.tensor_copy(out=APR, in_=ANGF))
            range_reduce(APR, ANGI, ANGF, 0, tQ)
            ac(lambda e: e.activation(out=APR, in_=APR, func=AF.Sin))
            dv(lambda e: e.tensor_copy(out=API, in_=ANG))
            range_reduce(API, ANGI, ANGF, 0, tQ)
            ac(lambda e: e.activation(out=API, in_=API, func=AF.Sin))
            dv(lambda e: e.tensor_tensor(out=APR, in0=APR, in1=MAGK, op=ALU.mult))
            dv(lambda e: e.tensor_tensor(out=API, in0=API, in1=MAGK, op=ALU.mult))
            dv(lambda e: e.tensor_copy(out=A1R, in_=APR[:, :, 8]))
            dv(lambda e: e.tensor_copy(out=A1I, in_=API[:, :, 8]))
            dv(lambda e: e.tensor_tensor(out=DEN, in0=LR, in1=LR, op=ALU.mult))
            dv(lambda e: e.tensor_tensor(out=T0, in0=LI, in1=LI, op=ALU.mult))
            dv(lambda e: e.tensor_tensor(out=DEN, in0=DEN, in1=T0, op=ALU.add))
            dv(lambda e: e.reciprocal(out=DEN, in_=DEN))
            dv(lambda e: e.tensor_scalar(out=T0, in0=A1R, scalar1=-1.0, scalar2=None, op0=ALU.add))
            dv(lambda e: e.tensor_tensor(out=FR, in0=T0, in1=LR, op=ALU.mult))
            dv(lambda e: e.tensor_tensor(out=T1, in0=A1I, in1=LI, op=ALU.mult))
            dv(lambda e: e.tensor_tensor(out=FR, in0=FR, in1=T1, op=ALU.add))
            dv(lambda e: e.tensor_tensor(out=FR, in0=FR, in1=DEN, op=ALU.mult))
            dv(lambda e: e.tensor_tensor(out=FI, in0=A1I, in1=LR, op=ALU.mult))
            dv(lambda e: e.tensor_tensor(out=T1, in0=T0, in1=LI, op=ALU.mult))
            dv(lambda e: e.tensor_tensor(out=FI, in0=FI, in1=T1, op=ALU.subtract))
            dv(lambda e: e.tensor_tensor(out=FI, in0=FI, in1=DEN, op=ALU.mult))
            bc = lambda a: a.unsqueeze(2).broadcast_to([128, 16, 16])
            dv(lambda e: e.tensor_tensor(out=TB0, in0=BR, in1=bc(FR), op=ALU.mult))
            dv(lambda e: e.tensor_tensor(out=TB1, in0=BI, in1=bc(FI), op=ALU.mult))
            dv(lambda e: e.tensor_tensor(out=BBR, in0=TB0, in1=TB1, op=ALU.subtract))
            dv(lambda e: e.tensor_tensor(out=TB0, in0=BI, in1=bc(FR), op=ALU.mult))
            dv(lambda e: e.tensor_tensor(out=TB1, in0=BR, in1=bc(FI), op=ALU.mult))
            dv(lambda e: e.tensor_tensor(out=BBI, in0=TB0, in1=TB1, op=ALU.add))
            for (CN, CT_) in ((CNR, CTR), (CNI, CTI)):
                p, tp = ps()
                for P in range(16):
                    S.op("pe", lambda e, P=P, p=p, CN=CN: e.transpose(out=p[:, P * 16:(P + 1) * 16], in_=CN[:, P, :], identity=IDF[0:16, 0:16]), tQ + (tCST,), (tp,))
                S.op("dve", lambda e, p=p, CT_=CT_: e.tensor_copy(out=CT_, in_=p[:, 0:256].rearrange("p (a b) -> p a b", a=16)), (tp,), tQ)
            p, tp = ps()
            S.op("pe", lambda e, p=p: e.matmul(p[:, 0:32], lhsT=CST[0:16, 986:1114], rhs=DM, start=True, stop=True), tQ + (tCST,), (tp,))
            S.op("dve", lambda e, p=p: e.tensor_copy(out=D8, in_=p[:, 0:32]), (tp,), tQ)
            W14 = W1.rearrange("p a (s h) -> p a s h", s=8); W24 = W2.rearrange("p a (s h) -> p a s h", s=8)

            def pw(AP_, lo, rev):
                v = AP_[:, :, lo:lo + 8]
                if rev:
                    v = AP_[:, :, lo + 7:lo - 1 if lo > 0 else None:-1]
                return v.unsqueeze(3).broadcast_to([128, 16, 8, 16])
            b4 = lambda a: a.unsqueeze(2).broadcast_to([128, 16, 8, 16])
            for s in range(8):
                k = 14 - s
                pr = lambda: APR[:, :, k:k + 1].broadcast_to([128, 16, 16])
                pi_ = lambda: API[:, :, k:k + 1].broadcast_to([128, 16, 16])
                dv(lambda e, s=s, k=k: e.tensor_tensor(out=W14[:, :, s, :], in0=BBR, in1=APR[:, :, k:k + 1].broadcast_to([128, 16, 16]), op=ALU.mult))
                dv(lambda e, s=s, k=k: e.tensor_tensor(out=W24[:, :, s, :], in0=BBI, in1=API[:, :, k:k + 1].broadcast_to([128, 16, 16]), op=ALU.mult))
            dv(lambda e: e.tensor_tensor(out=WTR, in0=W1, in1=W2, op=ALU.subtract))
            for s in range(8):
                k = 14 - s
                dv(lambda e, s=s, k=k: e.tensor_tensor(out=W14[:, :, s, :], in0=BBI, in1=APR[:, :, k:k + 1].broadcast_to([128, 16, 16]), op=ALU.mult))
                dv(lambda e, s=s, k=k: e.tensor_tensor(out=W24[:, :, s, :], in0=BBR, in1=API[:, :, k:k + 1].broadcast_to([128, 16, 16]), op=ALU.mult))
            dv(lambda e: e.tensor_tensor(out=WTI, in0=W1, in1=W2, op=ALU.add))
            for (WT_, WP_) in ((WTR, WPRE), (WTI, WPIM)):
                for half in range(2):
                    pt, tpt = pst()
                    for P8 in range(8):
                        P = half * 8 + P8
                        S.op("pe", lambda e, P=P, P8=P8, pt=pt, WT_=WT_: e.transpose(out=pt[:, P8 * 128:(P8 + 1) * 128], in_=WT_[:, P, :], identity=IDB), tQ + (tCST,), (tpt,))
                    S.op("dve", lambda e, half=half, pt=pt, WP_=WP_: e.tensor_copy(out=WP_[:, half * 8:(half + 1) * 8, :], in_=pt.rearrange("p (a b) -> p a b", a=8)), (tpt,), (tS5C,))
            for (dstR, dstI, k0) in ((VRE, VIM, 8), (XRB, XIB, 0)):
                for t in range(8):
                    k = k0 + t
                    dv(lambda e, t=t, k=k: e.tensor_tensor(out=W14[:, :, t, :], in0=CTR, in1=APR[:, :, k:k + 1].broadcast_to([128, 16, 16]), op=ALU.mult))
                    dv(lambda e, t=t, k=k: e.tensor_tensor(out=W24[:, :, t, :], in0=CTI, in1=API[:, :, k:k + 1].broadcast_to([128, 16, 16]), op=ALU.mult))
                S.op("dve", lambda e, dstR=dstR: e.tensor_tensor(out=dstR, in0=W1, in1=W2, op=ALU.subtract), tQ, tQ + (tS5C,))
                for t in range(8):
                    k = k0 + t
                    dv(lambda e, t=t, k=k: e.tensor_tensor(out=W14[:, :, t, :], in0=CTR, in1=API[:, :, k:k + 1].broadcast_to([128, 16, 16]), op=ALU.mult))
                    dv(lambda e, t=t, k=k: e.tensor_tensor(out=W24[:, :, t, :], in0=CTI, in1=APR[:, :, k:k + 1].broadcast_to([128, 16, 16]), op=ALU.mult))
                S.op("dve", lambda e, dstI=dstI: e.scalar_tensor_tensor(out=dstI, in0=W1, scalar=-1.0, in1=W2, op0=ALU.mult, op1=ALU.subtract), tQ, tQ + (tS5C,))
            for g in range(32):
                P, r = g // 2, g % 2
                p, tp = ps()
                S.op("pe", lambda e, P=P, r=r, p=p: e.matmul(p[:, 0:128], lhsT=WTR[r * 64:(r + 1) * 64, P, :], rhs=XRB[r * 64:(r + 1) * 64, P, :], start=True, stop=False), tQ, (tp,))
                S.op("pe", lambda e, P=P, r=r, p=p: e.matmul(p[:, 0:128], lhsT=WTI[r * 64:(r + 1) * 64, P, :], rhs=XIB[r * 64:(r + 1) * 64, P, :], start=False, stop=True), tQ, (tp,))
                S.op("dve", lambda e, p=p: e.tensor_tensor(out=W1[:, 0, :], in0=p[:, 0:128], in1=CST[:, 128:256], op=ALU.mult), (tp, tCST) + tQ, tQ)
                S.op("dve", lambda e, g=g: e.scalar_tensor_tensor(out=G5[:, g, :], in0=CST[:, 0:128], scalar=D8[:, g:g + 1], in1=W1[:, 0, :], op0=ALU.mult, op1=ALU.add),
                     tQ + (tCST,), (tS5C,))
            S.op("act", lambda e: e.activation(out=RHO8, in_=LRS, func=AF.Exp, scale=8.0), tQ, (tS5C,))
            M8 = CST[:, 778:842]
            dv(lambda e: e.tensor_tensor(out=ANGR, in0=TH.unsqueeze(2).broadcast_to([128, 16, 64]), in1=M8.unsqueeze(1).broadcast_to([128, 16, 64]), op=ALU.mult))
            dv(lambda e: e.tensor_scalar(out=ANGRF, in0=ANGR, scalar1=math.pi / 2, scalar2=None, op0=ALU.add))
            S.op("dve", lambda e: e.tensor_copy(out=COSR, in_=ANGRF), tQ, (tS5C,))
            range_reduce(COSR, ANGRI, ANGRF, 0, tQ + (tS5C,))
            S.op("act", lambda e: e.activation(out=COSR, in_=COSR, func=AF.Sin), (tS5C,), (tS5C,))
            S.op("dve", lambda e: e.tensor_copy(out=SINR, in_=ANGR), tQ, (tS5C,))
            range_reduce(SINR, ANGRI, ANGRF, 0, tQ + (tS5C,))
            S.op("act", lambda e: e.activation(out=SINR, in_=SINR, func=AF.Sin), (tS5C,), (tS5C,))
            S.barrier()

            load_gain(nmix_d[l:l + 1, :], 0); load_gain(ncross_d[l:l + 1, :], 1); load_gain(nffn_d[l:l + 1, :], 2)
            pending = []
            def do_tile(ti):
                r0 = ti * TT
                src = x_d if l == 0 else xs_d
                X, tX = XS[ti % 2], tXS[ti % 2]

                def xload(tj):
                    Xj, tXj = XS[tj % 2], tXS[tj % 2]
                    for st in range(NST):
                        S.dma("pool", lambda e, st=st: e.dma_start(out=Xj[:, st, :], in_=src[tj * TT + st * 128:tj * TT + (st + 1) * 128, :]), "x%d_%d" % (tj % 2, st), (), (tXj[st],))

                S.dma("pool", lambda e: e.dma_start(out=SIN2, in_=rot_d[ti, 0]), "rotl", (), (tROT,))
                S.dma("pool", lambda e: e.dma_start(out=COS2, in_=rot_d[ti, 1]), "rotl", (), (tROT,))
                while pending:
                    pending.pop(0)()
                if ti == 0:
                    xload(0)
                if ti + 1 < NT:
                    xload(ti + 1)

                norm_transpose(0, X, tX)
                wb, twb = wbuf()
                wload(wb, twb, "in", l, 0, 8, 512)
                for t2 in range(4):
                    p, tp = ps()
                    for kt in range(8):
                        for r in range(2):
                            t = 2 * t2 + r
                            S.op("pe", lambda e, t=t, r=r, kt=kt, p=p, wb=wb: e.matmul(p[r * 64:(r + 1) * 64, :], lhsT=XT[:, kt, t::8], rhs=wb[:, kt, :], start=(kt == 0), stop=(kt == 7)),
                                 (tXT, twb), (tp,))
                    for r in range(2):
                        t = 2 * t2 + r
                        S.op("dve", lambda e, t=t, r=r, p=p: e.tensor_copy(out=TMUv[0:64, :, t, :], in_=p[r * 64:(r + 1) * 64, :].rearrange("p (g h) -> p g h", g=32)), (tp,), (tTM,))
                for half in range(2):
                    pt, tpt = pst()
                    for g16 in range(16):
                        g = half * 16 + g16
                        S.op("pe", lambda e, g=g, g16=g16, pt=pt: e.transpose(out=pt[:, g16 * 64:(g16 + 1) * 64], in_=TMUv[0:64, g, :, :].rearrange("p t h -> p (t h)"), identity=IDB[0:64, 0:64]),
                             (tTM, tCST), (tpt,))
                    copy(ev_eng(), U8B[:, half * 16:(half + 1) * 16, :], pt.rearrange("p (g j) -> p g j", g=16), (tpt,), (tU8,))
                zb = []
                for half in range(2):
                    zr, tzr = ps()
                    zi, tzi = ps()
                    zb.append((zr, tzr, zi, tzi))
                    for P8 in range(8):
                        P = half * 8 + P8
                        for r in range(2):
                            g = 2 * P + r
                            S.op("pe", lambda e, P=P, P8=P8, r=r, g=g, zr=zr: e.matmul(zr[r * 64:(r + 1) * 64, P8 * 64:(P8 + 1) * 64], lhsT=WPRE[:, P, r * 64:(r + 1) * 64], rhs=U8B[:, g, :], start=True, stop=True),
                                 (tS5C, tU8), (tzr,))
                            S.op("pe", lambda e, P=P, P8=P8, r=r, g=g, zi=zi: e.matmul(zi[r * 64:(r + 1) * 64, P8 * 64:(P8 + 1) * 64], lhsT=WPIM[:, P, r * 64:(r + 1) * 64], rhs=U8B[:, g, :], start=True, stop=True),
                                 (tS5C, tU8), (tzi,))
                tA, tB, tC, tD = tSC[0], tSC[1], tSC[2], tSC[3]

                def halfv(half):
                    hs = slice(half * 8, (half + 1) * 8)
                    cosr = COSR[:, hs, :].rearrange("p a b -> p (a b)")
                    sinr = SINR[:, hs, :].rearrange("p a b -> p (a b)")
                    A_, B_, C_, D_ = (SC[i][:, half * 512:(half + 1) * 512] for i in range(4))
                    return hs, cosr, sinr, A_, B_, C_, D_
                for half in range(2):
                    hs, cosr, sinr, A_, B_, C_, D_ = halfv(half)
                    zr, tzr, zi, tzi = zb[half]
                    S.op("dve", lambda e, zr=zr, cosr=cosr, A_=A_: e.tensor_tensor(out=A_, in0=zr[:, :], in1=cosr, op=ALU.mult), (tzr, tS5C), (tA,))
                    S.op("dve", lambda e, zi=zi, sinr=sinr, B_=B_: e.tensor_tensor(out=B_, in0=zi[:, :], in1=sinr, op=ALU.mult), (tzi, tS5C), (tB,))
                    S.op("dve", lambda e, A_=A_, B_=B_: e.tensor_tensor(out=A_, in0=A_, in1=B_, op=ALU.add), (tA, tB), (tA,))
                    S.op("dve", lambda e, zi=zi, cosr=cosr, C_=C_: e.tensor_tensor(out=C_, in0=zi[:, :], in1=cosr, op=ALU.mult), (tzi, tS5C), (tC,))
                    S.op("dve", lambda e, zr=zr, sinr=sinr, B_=B_: e.tensor_tensor(out=B_, in0=zr[:, :], in1=sinr, op=ALU.mult), (tzr, tS5C, tA), (tB,))
                    S.op("dve", lambda e, C_=C_, B_=B_: e.tensor_tensor(out=C_, in0=C_, in1=B_, op=ALU.subtract), (tC, tB), (tC,))
                for half in range(2):
                    hs, cosr, sinr, A_, B_, C_, D_ = halfv(half)
                    for P8 in range(8):
                        P = half * 8 + P8
                        cs = slice(P8 * 64, (P8 + 1) * 64)
                        S.op("dve", lambda e, P=P, cs=cs, A_=A_, B_=B_: e.tensor_tensor_scan(out=B_[:, cs], data0=RHO8[:, P:P + 1].broadcast_to([128, 64]), data1=A_[:, cs],
                                                                                            initial=STRE[:, P:P + 1], op0=ALU.mult, op1=ALU.add), (tA, tS5C, tST, tC), (tB,))
                        S.op("dve", lambda e, P=P, cs=cs, C_=C_, D_=D_: e.tensor_tensor_scan(out=D_[:, cs], data0=RHO8[:, P:P + 1].broadcast_to([128, 64]), data1=C_[:, cs],
                                                                                            initial=STIM[:, P:P + 1], op0=ALU.mult, op1=ALU.add), (tC, tS5C, tST), (tD,))
                    S.op("dve", lambda e, A_=A_, B_=B_, cosr=cosr: e.tensor_tensor(out=A_, in0=B_, in1=cosr, op=ALU.mult), (tB, tS5C), (tA,))
                    S.op("dve", lambda e, C_=C_, D_=D_, sinr=sinr: e.tensor_tensor(out=C_, in0=D_, in1=sinr, op=ALU.mult), (tD, tS5C), (tC,))
                    S.op("dve", lambda e, A_=A_, C_=C_: e.tensor_tensor(out=A_, in0=A_, in1=C_, op=ALU.subtract), (tA, tC), (tA,))
                    S.op("dve", lambda e, C_=C_, D_=D_, cosr=cosr: e.tensor_tensor(out=C_, in0=D_, in1=cosr, op=ALU.mult), (tD, tS5C, tA), (tC,))
                    S.op("dve", lambda e, B_=B_, sinr=sinr: e.tensor_tensor(out=B_, in0=B_, in1=sinr, op=ALU.mult), (tB, tS5C), (tB,))
                    S.op("dve", lambda e, C_=C_, B_=B_: e.tensor_tensor(out=C_, in0=C_, in1=B_, op=ALU.add), (tC, tB), (tC,))
                    A3 = A_.rearrange("p (a b) -> p a b", a=8); C3 = C_.rearrange("p (a b) -> p a b", a=8)
                    S.op("dve", lambda e, hs=hs: e.tensor_copy(out=SPRE[:, hs, 0:1], in_=STRE[:, hs].unsqueeze(2)), (tST,), (tSP,))
                    S.op("dve", lambda e, hs=hs: e.tensor_copy(out=SPIM[:, hs, 0:1], in_=STIM[:, hs].unsqueeze(2)), (tST,), (tSP,))
                    S.op("dve", lambda e, hs=hs, A3=A3: e.tensor_copy(out=SPRE[:, hs, 1:64], in_=A3[:, :, 0:63]), (tA,), (tSP,))
                    S.op("dve", lambda e, hs=hs, C3=C3: e.tensor_copy(out=SPIM[:, hs, 1:64], in_=C3[:, :, 0:63]), (tC,), (tSP,))
                    S.op("dve", lambda e, hs=hs, A3=A3: e.tensor_copy(out=STRE[:, hs].unsqueeze(2), in_=A3[:, :, 63:64]), (tA, tSP), (tST,))
                    S.op("dve", lambda e, hs=hs, C3=C3: e.tensor_copy(out=STIM[:, hs].unsqueeze(2), in_=C3[:, :, 63:64]), (tC, tSP), (tST,))
                RA, RB_ = SC[4][:, 0:512], SC[5][:, 0:512]
                tRA = (tSC[4], tSC4b); tRBt = (tSC[5],)
                for (ci, cis, DST, tD_) in ((1, 5, QT, tQT), (2, 6, KT, tKT)):
                    wa, twa = wbuf(); wload(wa, twa, "in", l, ci * 512, 8, 512)
                    wsb, twsb = wbuf(); wload(wsb, twsb, "in", l, cis * 512, 8, 512)
                    for h in range(4):
                        p1, tp1 = ps(); p2, tp2 = ps()
                        for kt in range(8):
                            S.op("pe", lambda e, h=h, kt=kt, p1=p1, wa=wa: e.matmul(p1[:, :], lhsT=wa[:, kt, h * 128:(h + 1) * 128], rhs=XT[:, kt, :], start=(kt == 0), stop=(kt == 7)), (twa, tXT), (tp1,))
                        for kt in range(8):
                            S.op("pe", lambda e, h=h, kt=kt, p2=p2, wsb=wsb: e.matmul(p2[:, :], lhsT=wsb[:, kt, h * 128:(h + 1) * 128], rhs=XT[:, kt, :], start=(kt == 0), stop=(kt == 7)), (twsb, tXT), (tp2,))
                        S.op("act", lambda e, p1=p1: e.activation(out=RA, in_=p1[:, :], func=AF.Copy), (tp1,), tRA)
                        S.op("act", lambda e, p2=p2: e.activation(out=RB_, in_=p2[:, :], func=AF.Copy), (tp2,), tRBt)
                        S.op("pool", lambda e: e.tensor_tensor(out=RA, in0=RA, in1=COS2, op=ALU.mult), tRA + (tROT,), tRA)
                        S.op("pool", lambda e: e.tensor_tensor(out=RB_, in0=RB_, in1=SIN2, op=ALU.mult), tRBt + (tROT,), tRBt)
                        S.op("pool", lambda e, h=h, DST=DST: e.tensor_tensor(out=DST[:, h, :], in0=RA, in1=RB_, op=ALU.add), tRA + tRBt, (tD_,))
                for (ci, is_g) in ((3, False), (4, True)):
                    wa, twa = wbuf(); wload(wa, twa, "in", l, ci * 512, 8, 512)
                    for st in range(NST):
                        p, tp = ps()
                        for kt in range(8):
                            S.op("pe", lambda e, st=st, kt=kt, p=p, wa=wa: e.matmul(p[:, :], lhsT=XT[:, kt, st * 128:(st + 1) * 128], rhs=wa[:, kt, :], start=(kt == 0), stop=(kt == 7)), (twa, tXT), (tp,))
                        if not is_g:
                            copy("act", VV[:, st, :], p[:, :], (tp,), (tVV,))
                        else:
                            S.op("act", lambda e, p=p: e.activation(out=RB_, in_=p[:, :], func=AF.Silu), (tp,), tRBt)
                            S.op("pool", lambda e, st=st: e.tensor_tensor(out=GG[:, st, :], in0=RB_, in1=GRET, op=ALU.mult), tRBt + (tGS,), (tGG,))
                for g4 in range(8):
                    p, tp = ps()
                    for gi in range(4):
                        g = g4 * 4 + gi
                        P, r = g // 2, g % 2
                        o = p[0:64, gi * 128:(gi + 1) * 128]
                        S.op("pe", lambda e, g=g, o=o: e.matmul(o, lhsT=U8B[:, g, :], rhs=G5[:, g, :], start=True, stop=False), (tU8, tS5C), (tp,))
                        S.op("pe", lambda e, P=P, r=r, o=o: e.matmul(o, lhsT=SPRE[r * 64:(r + 1) * 64, P, :], rhs=VRE[r * 64:(r + 1) * 64, P, :], start=False, stop=False), (tSP, tS5C), (tp,))
                        S.op("pe", lambda e, P=P, r=r, o=o: e.matmul(o, lhsT=SPIM[r * 64:(r + 1) * 64, P, :], rhs=VIM[r * 64:(r + 1) * 64, P, :], start=False, stop=True), (tSP, tS5C), (tp,))
                    S.op("act", lambda e, g4=g4, p=p: e.activation(out=TMUZ[0:64, :, g4 * 64:(g4 + 1) * 64].rearrange("p t (g n) -> p g t n", g=4),
                                                                    in_=p[0:64, :].rearrange("p (g t n) -> p g t n", g=4, t=8), func=AF.Gelu), (tp, tU8), (tTM,))
                for ct in range(4):
                    pt, tpt = pst()
                    for t in range(8):
                        S.op("pe", lambda e, ct=ct, t=t, pt=pt: e.transpose(out=pt[:, t * 64:(t + 1) * 64], in_=TMUZ[0:64, t, ct * 128:(ct + 1) * 128], identity=IDB[0:64, 0:64]),
                             (tTM, tCST), (tpt,))
                    copy(ev_eng(), ZT[:, ct, :].rearrange("p (j t) -> p j t", t=8), pt[:, 0:512].rearrange("p (t j) -> p j t", t=8), (tpt,), (tZT,))
                ZZ = SC[0].bitcast(BF16)
                SQ = SC[1].bitcast(BF16)
                for ct2 in range(4):
                    p, tp = ps()
                    for ct in range(4):
                        S.op("pe", lambda e, ct=ct, ct2=ct2, p=p: e.matmul(p[:, :], lhsT=WGLU[:, ct, ct2 * 128:(ct2 + 1) * 128], rhs=ZT[:, ct, :], start=(ct == 0), stop=(ct == 3)),
                             (tWGLU, tZT), (tp,))
                    S.op("act", lambda e, ct2=ct2, p=p: e.activation(out=SC[2][:, 0:512], in_=p[:, :], func=AF.Sigmoid, bias=BGLU[:, ct2:ct2 + 1]), (tp, tGS), (tSC[2],))
                    S.op("dve", lambda e, ct2=ct2: e.tensor_tensor(out=ZZ[:, ct2 * 512:(ct2 + 1) * 512], in0=ZT[:, ct2, :], in1=SC[2][:, 0:512], op=ALU.mult), (tZT, tSC[2]), (tSC[0],))
                    S.op("dve", lambda e, ct2=ct2: e.tensor_tensor(out=SQ[:, ct2 * 512:(ct2 + 1) * 512], in0=ZZ[:, ct2 * 512:(ct2 + 1) * 512], in1=ZZ[:, ct2 * 512:(ct2 + 1) * 512], op=ALU.mult),
                         (tSC[0],), (tSC[1],))
                p, tp = ps()
                for ct in range(4):
                    S.op("pe", lambda e, ct=ct, p=p: e.matmul(p[:, :], lhsT=ONESB, rhs=SQ[:, ct * 512:(ct + 1) * 512], start=(ct == 0), stop=(ct == 3)), (tSC[1], tCST), (tp,))
                S.op("act", lambda e, p=p: e.activation(out=SC[3][:, 0:512], in_=p[:, :], func=AF.Sqrt, scale=1.0 / 512, bias=EPS), (tp,), (tSC[3],))
                S.op("dve", lambda e: e.reciprocal(out=SC[3][:, 0:512], in_=SC[3][:, 0:512]), (tSC[3],), (tSC[3],))
                for ct in range(4):
                    S.op("dve", lambda e, ct=ct: e.scalar_tensor_tensor(out=YT[:, ct, :], in0=ZZ[:, ct * 512:(ct + 1) * 512], scalar=GS5[:, ct:ct + 1], in1=SC[3][:, 0:512], op0=ALU.mult, op1=ALU.mult),
                         (tSC[0], tSC[3], tGS), (tYT,))

                KTM4 = SC[0].bitcast(BF16).rearrange("p (s h d) -> p s h d", s=4, h=4); tKTM = tSC[0]
                PT4 = SC[1].bitcast(BF16).rearrange("p (s h d) -> p s h d", s=4, h=4); tPT = tSC[1]
                RB4 = SC[2].bitcast(BF16).rearrange("p (s h d) -> p s h d", s=4, h=4); tRB = tSC[2]
                OO2 = [SC[3][:, i * 512:(i + 1) * 512].rearrange("p (h d) -> p h d", h=4) for i in range(2)]; tOO2 = [tSC[3], tSC3b]
                YR2 = [SC[4].bitcast(BF16)[:, i * 512:(i + 1) * 512] for i in range(2)]; tYR2 = [tSC[4], tSC4b]
                BNa = [SMALL[:, 8 + i * 24:8 + (i + 1) * 24] for i in range(2)]; tBN = tSMALL
                for st in range(NST):
                    cs = slice(st * 128, (st + 1) * 128)
                    pt, tpt = pst()
                    for h in range(4):
                        S.op("pe", lambda e, h=h, cs=cs, pt=pt: e.transpose(out=pt[:, h * 128:(h + 1) * 128], in_=KT[:, h, cs], identity=IDB), (tKT, tCST), (tpt,))
                    for h in range(4):
                        S.op("act", lambda e, h=h, st=st, pt=pt: e.activation(out=KTM4[:, st, h, :], in_=pt[:, h * 128:(h + 1) * 128], func=AF.Copy, scale=CST[:, 772 + h:773 + h]),
                             (tpt, tCST), (tKTM,))
                    p, tp = ps()
                    for h in range(4):
                        S.op("pe", lambda e, h=h, cs=cs, p=p: e.matmul(p[:, h * 128:(h + 1) * 128], lhsT=KT[:, h, cs], rhs=QT[:, h, cs], start=True, stop=True), (tKT, tQT), (tp,))
                    S.op("dve", lambda e, st=st, p=p: e.tensor_tensor(out=PT4[:, st].rearrange("p h d -> p (h d)"), in0=p[:, :], in1=CST[:, 256:768], op=ALU.mult), (tp, tCST), (tPT,))
                    pk, tpk = ps()
                    for h in range(4):
                        S.op("pe", lambda e, h=h, st=st, pk=pk: e.matmul(pk[:, h * 128:(h + 1) * 128], lhsT=KTM4[:, st, h, :], rhs=VV[:, st, h * 128:(h + 1) * 128], start=True, stop=True), (tKTM, tVV), (tpk,))
                    S.op("dve", lambda e, st=st: e.tensor_copy(out=RB4[:, st], in_=RST), (tR,), (tRB,))
                    S.op("dve", lambda e: e.tensor_tensor(out=RST, in0=RST, in1=G128T, op=ALU.mult), (tR, tRB, tCST), (tR,))
                    S.op("dve", lambda e, pk=pk: e.tensor_tensor(out=RST.rearrange("p h d -> p (h d)"), in0=pk[:, :], in1=RST.rearrange("p h d -> p (h d)"), op=ALU.add), (tpk, tR), (tR,))

                def ret_o(st):
                    cs = slice(st * 128, (st + 1) * 128)
                    OO, tOO = OO2[st % 2], tOO2[st % 2]
                    YR, tYR = YR2[st % 2], tYR2[st % 2]
                    BN = BNa[st % 2]
                    po, tpo = ps()
                    for h in range(4):
                        S.op("pe", lambda e, h=h, po=po: e.matmul(po[:, h * 128:(h + 1) * 128], lhsT=PT4[:, st, h, :], rhs=VV[:, st, h * 128:(h + 1) * 128], start=True, stop=False), (tPT, tVV), (tpo,))
                        S.op("pe", lambda e, h=h, po=po: e.matmul(po[:, h * 128:(h + 1) * 128], lhsT=QT[:, h, cs], rhs=RB4[:, st, h, :], start=False, stop=True), (tQT, tRB), (tpo,))
                    for h in range(4):
                        S.op("act", lambda e, h=h, po=po: e.activation(out=OO[:, h, :], in_=po[:, h * 128:(h + 1) * 128], func=AF.Copy, scale=CST[:, 768 + h:769 + h]), (tpo, tCST), (tOO,))
                    OOf = OO.rearrange("p h d -> p (h d)")
                    SQt = SC[5][:, 0:512]
                    MV = MVa[st % 2]; RS_ = RSa[st % 2]
                    S1 = MV[:, 0:4]; S2 = MV[:, 4:8]
                    S.op("dve", lambda e: e.tensor_reduce(out=S1, in_=OO, axis=AX.X, op=ALU.add), (tOO,), (tBN,))
                    S.op("dve", lambda e: e.tensor_tensor(out=SQt, in0=OOf, in1=OOf, op=ALU.mult), (tOO,), (tSC[5],))
                    S.op("dve", lambda e: e.tensor_reduce(out=S2, in_=SQt.rearrange("p (h d) -> p h d", h=4), axis=AX.X, op=ALU.add), (tSC[5],), (tBN,))
                    S.op("dve", lambda e: e.tensor_scalar(out=S1, in0=S1, scalar1=1.0 / 128.0, scalar2=None, op0=ALU.mult), (tBN,), (tBN,))
                    S.op("dve", lambda e: e.tensor_tensor(out=RS_, in0=S1, in1=S1, op=ALU.mult), (tBN,), (tBN,))
                    S.op("dve", lambda e: e.scalar_tensor_tensor(out=RS_, in0=S2, scalar=1.0 / 128.0, in1=RS_, op0=ALU.mult, op1=ALU.subtract), (tBN,), (tBN,))
                    S.op("act", lambda e: e.activation(out=RS_, in_=RS_, func=AF.Sqrt, bias=EPS), (tBN,), (tBN,))
                    S.op("dve", lambda e: e.reciprocal(out=RS_, in_=RS_), (tBN,), (tBN,))
                    S.op("dve", lambda e: e.tensor_tensor(out=OO, in0=OO, in1=S1.unsqueeze(2).broadcast_to([128, 4, 128]), op=ALU.subtract), (tOO, tBN), (tOO,))
                    S.op("dve", lambda e: e.tensor_tensor(out=OO, in0=OO, in1=RS_.unsqueeze(2).broadcast_to([128, 4, 128]), op=ALU.mult), (tOO, tBN), (tOO,))
                    S.op("dve", lambda e: e.tensor_tensor(out=YR, in0=OOf, in1=GG[:, st, :], op=ALU.mult), (tOO, tGG), (tYR,))

                def ret_t(st):
                    cs = slice(st * 128, (st + 1) * 128)
                    YR, tYR = YR2[st % 2], tYR2[st % 2]
                    pt, tpt = pst()
                    for h in range(4):
                        S.op("pe", lambda e, h=h, pt=pt: e.transpose(out=pt[:, h * 128:(h + 1) * 128], in_=YR[:, h * 128:(h + 1) * 128], identity=IDB), (tYR, tCST), (tpt,))
                    copy("act", YT[:, 4:8, cs], pt[:, 0:512].rearrange("p (h t) -> p h t", h=4), (tpt,), (tYT,))
                ret_o(0)
                for st in range(1, NST):
                    ret_o(st)
                    ret_t(st - 1)
                ret_t(NST - 1)
                proj_tm_add(YT, (tYT,), "out", l, 8, X, tX)

                for _ in range(pc_per_tile):
                    if pcq:
                        pcq.pop(0)()
                norm_transpose(1, X, tX)
                for cc in range(2):
                    wa, twa = wbuf(); wload(wa, twa, "cq", l, cc * 512, 8, 512)
                    for c4 in range(4):
                        p, tp = ps()
                        for kt in range(8):
                            S.op("pe", lambda e, c4=c4, kt=kt, p=p, wa=wa: e.matmul(p[:, :], lhsT=wa[:, kt, c4 * 128:(c4 + 1) * 128], rhs=XT[:, kt, :], start=(kt == 0), stop=(kt == 7)), (twa, tXT), (tp,))
                        S.op("act", lambda e, cc=cc, c4=c4, p=p: e.activation(out=QC[:, cc * 4 + c4, :], in_=p[:, :], func=AF.Copy, scale=1.0 / 16.0), (tp,), tuple(tQC))
                MX = SMALL[:, 0:4]; SM = RSa[0]; tSM = tBN

                def xa_scores(st):
                    cs = slice(st * 128, (st + 1) * 128)
                    pa, tpa = ps(); pb, tpb = ps()
                    for h in range(4):
                        pp, tpp = (pa, tpa) if h < 2 else (pb, tpb)
                        o = pp[:, (h % 2) * 256:(h % 2 + 1) * 256]
                        for hf in range(2):
                            S.op("pe", lambda e, h=h, hf=hf, o=o: e.matmul(o, lhsT=QC[:, 2 * h + hf, cs], rhs=KM[:, 2 * h + hf, :], start=(hf == 0), stop=(hf == 1)), tuple(tQC) + (tKV,), (tpp,))
                    return ((pa, tpa), (pb, tpb))

                def xa_softmax(st, banks):
                    PNb, tPNb = PN2[st % 2], tPN2[st % 2]
                    for i, (pp, tpp) in enumerate(banks):
                        S.op("dve", lambda e, i=i, pp=pp: e.tensor_reduce(out=MX[:, i:i + 1], in_=pp[:, :], axis=AX.X, op=ALU.max), (tpp,), (tSMALL,))
                    S.op("dve", lambda e: e.tensor_tensor(out=MX[:, 2:3], in0=MX[:, 0:1], in1=MX[:, 1:2], op=ALU.max), (tSMALL,), (tSMALL,))
                    S.op("dve", lambda e: e.tensor_scalar(out=MX[:, 3:4], in0=MX[:, 2:3], scalar1=-1.0, scalar2=None, op0=ALU.mult), (tSMALL,), (tSMALL,))
                    for h in range(4):
                        pp, tpp = banks[h // 2]
                        S.op("act", lambda e, h=h, pp=pp: e.activation(out=EX[:, h * 256:(h + 1) * 256], in_=pp[:, (h % 2) * 256:(h % 2 + 1) * 256], func=AF.Exp, bias=MX[:, 3:4], accum_out=SM[:, h:h + 1]),
                             (tpp, tSMALL), tuple(tEX) + (tSM,))
                    S.op("dve", lambda e: e.reciprocal(out=SM, in_=SM), (tSM,), (tSM,))
                    S.op("pool", lambda e: e.tensor_tensor(out=PNb.rearrange("p (h m) -> p h m", h=4), in0=EX.rearrange("p (h m) -> p h m", h=4), in1=SM.unsqueeze(2).broadcast_to([128, 4, 256]), op=ALU.mult),
                         tuple(tEX) + (tSM,), (tPNb,))

                def xa_pv(st):
                    cs = slice(st * 128, (st + 1) * 128)
                    PNb, tPNb = PN2[st % 2], tPN2[st % 2]
                    pt, tpt = pst()
                    for i8 in range(8):
                        S.op("pe", lambda e, i8=i8, pt=pt: e.transpose(out=pt[:, i8 * 128:(i8 + 1) * 128], in_=PNb[:, i8 * 128:(i8 + 1) * 128], identity=IDB), (tPNb, tCST), (tpt,))
                    copy("act", PNT, pt.rearrange("p (a b) -> p a b", a=8), (tpt,), tuple(tPNT))
                    for half in range(2):
                        po, tpo = ps()
                        for h2 in range(2):
                            h = half * 2 + h2
                            for eh in range(2):
                                o = po[:, (h2 * 2 + eh) * 128:(h2 * 2 + eh + 1) * 128]
                                for mh in range(2):
                                    S.op("pe", lambda e, h=h, eh=eh, mh=mh, o=o: e.matmul(o, lhsT=VM[:, mh, h * 256 + eh * 128:h * 256 + (eh + 1) * 128], rhs=PNT[:, 2 * h + mh, :], start=(mh == 0), stop=(mh == 1)),
                                         (tKV,) + tuple(tPNT), (tpo,))
                        copy(ev_eng(), OT[:, half * 4:(half + 1) * 4, cs], po[:, :].rearrange("p (a b) -> p a b", a=4), (tpo,), tuple(tOT))
                bk = xa_scores(0)
                for st in range(NST):
                    nbk = xa_scores(st + 1) if st + 1 < NST else None
                    xa_softmax(st, bk)
                    xa_pv(st)
                    bk = nbk
                proj_tm_add(OT, tuple(tOT), "co", l, 8, X, tX)

                norm_transpose(2, X, tX)
                for fc in range(11):
                    wa, twa = wbuf()
                    S.dma("sp", lambda e, fc=fc, wa=wa: e.dma_start(out=wa[:, :, 0:256], in_=wbf["g"][l, fc]), wkey[id(twa)], (tW[("g", l)],), (twa,))
                    S.dma("sp", lambda e, fc=fc, wa=wa: e.dma_start(out=wa[:, :, 256:512], in_=wbf["u"][l, fc]), wkey[id(twa)], (tW[("u", l)],), (twa,))
                    for f2 in range(2):
                        f = fc * 2 + f2
                        pg, tpg = ps(); pu, tpu = ps()
                        for kt in range(8):
                            S.op("pe", lambda e, f2=f2, kt=kt, pg=pg, wa=wa: e.matmul(pg[:, :], lhsT=wa[:, kt, f2 * 128:(f2 + 1) * 128], rhs=XT[:, kt, :], start=(kt == 0), stop=(kt == 7)), (twa, tXT), (tpg,))
                        for kt in range(8):
                            S.op("pe", lambda e, f2=f2, kt=kt, pu=pu, wa=wa: e.matmul(pu[:, :], lhsT=wa[:, kt, 256 + f2 * 128:256 + (f2 + 1) * 128], rhs=XT[:, kt, :], start=(kt == 0), stop=(kt == 7)), (twa, tXT), (tpu,))
                        S.op("act", lambda e, pg=pg: e.activation(out=SC[5][:, 0:512], in_=pg[:, :], func=AF.Silu), (tpg,), (tSC[5],))
                        S.op("dve", lambda e, f=f, pu=pu: e.tensor_tensor(out=H[:, f, :], in0=SC[5][:, 0:512], in1=pu[:, :], op=ALU.mult), (tpu, tSC[5]), tuple(tH[:-1]))
                for c4 in range(4):
                    wd_, twd = WD[c4], tWDs[c4]
                    S.dma("sp", lambda e, c4=c4, wd_=wd_: e.dma_start(out=wd_, in_=wbf["d"][l, c4]), "wd%d" % c4, (tW[("d", l)],), twd)
                    for st in range(NST):
                        p, tp = ps()
                        for f in range(NF):
                            S.op("pe", lambda e, st=st, f=f, p=p, wd_=wd_: e.matmul(p[:, 0:256], lhsT=H[:, f, st * 128:(st + 1) * 128], rhs=wd_[:, f, :], start=(f == 0), stop=(f == NF - 1)),
                                 tuple(tH[:-1]) + twd, (tp,))
                        S.op("dve", lambda e, st=st, c4=c4, p=p: e.tensor_tensor(out=X[:, st, c4 * 256:(c4 + 1) * 256], in0=p[:, 0:256], in1=X[:, st, c4 * 256:(c4 + 1) * 256], op=ALU.add),
                             (tp, tX[st]), (tX[st],))

                def emit_out():
                    if l < NL - 1:
                        for st in range(NST):
                            S.dma("pool", lambda e, st=st: e.dma_start(out=xs_d[r0 + st * 128:r0 + (st + 1) * 128, :], in_=X[:, st, :]), "xo%d" % st, (tX[st],), ())
                    else:
                        for st in range(NST):
                            S.dma("pool", lambda e, st=st: e.dma_start(out=out_d[r0 + st * 128:r0 + (st + 1) * 128, :], in_=X[:, st, :]), "xo%d" % st, (tX[st],), ())
                if l == NL - 1:
                    if ti == 0:
                        S.dma("pool", lambda e: e.dma_start(out=GFIN, in_=nfin_d[0:1, :].partition_broadcast(128)), "gfin", (), (tGFIN,))
                    for st in range(NST):
                        S.op("act", lambda e, st=st: e.activation(out=JUNK, in_=X[:, st, :], func=AF.Square, accum_out=SS[:, st:st + 1]), (tX[st],), (tSC[5], tSS))
                    S.op("dve", lambda e: e.tensor_scalar(out=SS[:, 4:8], in0=SS[:, 0:4], scalar1=1.0 / D, scalar2=EPS, op0=ALU.mult, op1=ALU.add), (tSS,), (tSS,))
                    S.op("act", lambda e: e.activation(out=SS[:, 4:8], in_=SS[:, 4:8], func=AF.Sqrt), (tSS,), (tSS,))
                    S.op("dve", lambda e: e.reciprocal(out=SS[:, 4:8], in_=SS[:, 4:8]), (tSS,), (tSS,))
                    for st in range(NST):
                        S.op("dve", lambda e, st=st: e.scalar_tensor_tensor(out=X[:, st, :], in0=X[:, st, :], scalar=SS[:, 4 + st:5 + st], in1=GFIN, op0=ALU.mult, op1=ALU.mult),
                             (tX[st], tSS, tGFIN), (tX[st],))
                pending.append(emit_out)

            for ti in range(NT):
                do_tile(ti)
            while pending:
                pending.pop(0)()
            while pcq:
                pcq.pop(0)()

        for l in range(NL):
            do_layer(l)
        S.barrier()
        print("instr counts:", {e: len(S.prog[e]) for e in ENG})
        block = stack.enter_context(nc.Block())
        S.emit(block)
    return nc


def make_inmap(inputs, b, NT, NL):
    T = NT * TT
    f = lambda a: np.ascontiguousarray(np.asarray(a, dtype=np.float32))
    w_in = np.asarray(inputs["w_in"], dtype=np.float32)[:NL]
    swap = np.concatenate([np.arange(h * 128 + 64, h * 128 + 128).tolist() + np.arange(h * 128, h * 128 + 64).tolist() for h in range(4)]).astype(np.int64)
    qs = w_in[:, :, 512:1024][:, :, swap]
    ks = w_in[:, :, 1024:1536][:, :, swap]
    w_in_ext = np.ascontiguousarray(np.concatenate([w_in, qs, ks], axis=2))
    m = {
        "x": f(inputs["x"][b, :T]),
        "pos": np.ascontiguousarray(np.asarray(inputs["positions"])[b:b + 1, :T].astype(np.int32)),
        "mem": f(inputs["mem"][b]),
        "norm_mix": f(inputs["norm_mix"][:NL]), "norm_cross": f(inputs["norm_cross"][:NL]),
        "norm_mem": f(inputs["norm_mem"][:NL]), "norm_ffn": f(inputs["norm_ffn"][:NL]),
        "norm_final": f(np.asarray(inputs["norm_final"]).reshape(1, D)),
        "w_in": w_in_ext,
        "lam_re": f(inputs["s5_lambda_re"][:NL]), "lam_im": f(inputs["s5_lambda_im"][:NL]), "log_step": f(inputs["s5_log_step"][:NL]),
        "b_re": f(inputs["s5_b_re"][:NL]), "b_im": f(inputs["s5_b_im"][:NL]),
        "c_re": f(inputs["s5_c_re"][:NL]), "c_im": f(inputs["s5_c_im"][:NL]),
        "s5_d": f(inputs["s5_d"][:NL]), "w_glu": f(inputs["s5_w_glu"][:NL]), "b_glu": f(inputs["s5_b_glu"][:NL]),
        "s5_out_norm": f(inputs["s5_out_norm"][:NL]), "ret_out_norm": f(np.asarray(inputs["ret_out_norm"])[:NL].reshape(NL, 512)),
        "w_out": f(inputs["w_out"][:NL]), "w_cq": f(inputs["w_cq"][:NL]), "w_ck": f(inputs["w_ck"][:NL]),
        "w_cv": f(inputs["w_cv"][:NL]), "w_co": f(inputs["w_co"][:NL]),
        "w_gate": f(inputs["w_gate"][:NL]), "w_up": f(inputs["w_up"][:NL]), "w_down": f(inputs["w_down"][:NL]),
        "cst": host_consts(),
    }
    return m


def kernel(**inputs):
    NT, NL = 16, 4
    nc = build_program(NT, NL)
    base = make_inmap(inputs, 0, NT, NL)
    in_maps = []
    for b in range(4):
        m = dict(base)
        m["x"] = np.ascontiguousarray(np.asarray(inputs["x"][b], dtype=np.float32))
        m["pos"] = np.ascontiguousarray(np.asarray(inputs["positions"])[b:b + 1].astype(np.int32))
        m["mem"] = np.ascontiguousarray(np.asarray(inputs["mem"][b], dtype=np.float32))
        in_maps.append(m)
    res = run_bass_kernel_spmd(nc, in_maps, core_ids=list(range(4)))
    out = np.stack([np.asarray(r["out"], dtype=np.float32) for r in res.results], axis=0)
    return out
```
